# Optimizing a Trainium2 kernel written in Bass

```python
import jax, jax.numpy as jnp
from jax import lax
import numpy as np


D_MODEL = 1024
BATCH = 4
SEQ = 8192
DEPTH = 1

CHUNK = 64
ROPE_THETA = 10000.0
LN_EPS = 1e-5
DSA_WIDTH = D_MODEL // 2
DSA_HEAD_DIM = 64
DSA_HEADS = DSA_WIDTH // DSA_HEAD_DIM
IDX_HEADS = 8
IDX_DIM = 32
IDX_SCALE = (IDX_HEADS * IDX_DIM) ** -0.5
DSA_TOPK_MAX = 256
Q_BLOCK = CHUNK
GLA_WIDTH = D_MODEL - DSA_WIDTH
GLA_HEADS = 4
GLA_DV = GLA_WIDTH // GLA_HEADS
GLA_DK = GLA_DV // 2
GLA_GATE_RANK = 16
GLA_TAU = 16.0
N_GROUPS = 4
EXPERTS_PER_GROUP = 8
N_EXPERTS = N_GROUPS * EXPERTS_PER_GROUP
TOP_K_INNER = 2
D_EXPERT = 512
DEEPNORM_ALPHA = (2.0 * DEPTH) ** 0.25
DEEPNORM_BETA = (8.0 * DEPTH) ** -0.25
IN_SIZES = (DSA_WIDTH, DSA_WIDTH, DSA_WIDTH, IDX_HEADS * IDX_DIM, IDX_DIM, IDX_HEADS,
            GLA_HEADS * GLA_DK, GLA_HEADS * GLA_DK, GLA_WIDTH, GLA_WIDTH, GLA_GATE_RANK)
IN_WIDTH = sum(IN_SIZES)

kernel_name = 'hybrid_dsa_gla_hmoe_block'


def layer_norm(x, g, b):
    xf = x.astype(jnp.float32)
    mu = jnp.mean(xf, axis=-1, keepdims=True)
    var = jnp.mean(jnp.square(xf - mu), axis=-1, keepdims=True)
    return ((xf - mu) * lax.rsqrt(var + LN_EPS) * g + b).astype(x.dtype)


def rope(x, pos):
    d = x.shape[-1]
    half = d // 2
    inv = 1.0 / (ROPE_THETA ** (jnp.arange(half, dtype=jnp.float32) / half))
    ang = pos.astype(jnp.float32)[:, None] * inv[None, :]
    cos = jnp.cos(ang)[:, None, :].astype(x.dtype)
    sin = jnp.sin(ang)[:, None, :].astype(x.dtype)
    x1, x2 = x[..., :half], x[..., half:]
    return jnp.concatenate([x1 * cos - x2 * sin, x2 * cos + x1 * sin], axis=-1)


def dsa_mixer(q, k, v, iq, ik, iw):
    B, T, H, dh = q.shape
    topk = min(DSA_TOPK_MAX, T // 4)
    nb = T // Q_BLOCK
    key_chunk = jnp.arange(T) // CHUNK

    def block(args):
        qb, iqb, iwb, start = args
        qpos = start + jnp.arange(Q_BLOCK)
        adm = key_chunk[None, :] <= (qpos // CHUNK)[:, None]
        rel = jax.nn.relu(jnp.einsum('bqhd,bsd->bqhs', iqb, ik))
        score = jnp.einsum('bqhs,bqh->bqs', rel, iwb).astype(jnp.float32)
        score = jnp.where(adm[None], score, -jnp.inf)
        vals, idx = lax.top_k(score, topk)
        ksel = jax.vmap(lambda kb, ib: kb[ib])(k, idx)
        vsel = jax.vmap(lambda vb, ib: vb[ib])(v, idx)
        s = jnp.einsum('bqhd,bqkhd->bhqk', qb, ksel).astype(jnp.float32) * (dh ** -0.5)
        s = jnp.where(jnp.isfinite(vals)[:, None], s, -jnp.inf)
        p = jax.nn.softmax(s, axis=-1).astype(v.dtype)
        return jnp.einsum('bhqk,bqkhd->bqhd', p, vsel)

    qbs = q.reshape(B, nb, Q_BLOCK, H, dh).transpose(1, 0, 2, 3, 4)
    iqbs = iq.reshape(B, nb, Q_BLOCK, IDX_HEADS, IDX_DIM).transpose(1, 0, 2, 3, 4)
    iwbs = iw.reshape(B, nb, Q_BLOCK, IDX_HEADS).transpose(1, 0, 2, 3)
    starts = jnp.arange(nb, dtype=jnp.int32) * Q_BLOCK
    out = lax.map(block, (qbs, iqbs, iwbs, starts))
    return out.transpose(1, 0, 2, 3, 4).reshape(B, T, H * dh)


def gla_mixer(q, k, v, log_a):
    B, T, H, dk = q.shape
    dv = v.shape[-1]
    N, C = T // CHUNK, CHUNK
    qf = q.astype(jnp.float32).reshape(B, N, C, H, dk) * (dk ** -0.5)
    kf = k.astype(jnp.float32).reshape(B, N, C, H, dk)
    vf = v.astype(jnp.float32).reshape(B, N, C, H, dv)
    b = jnp.cumsum(log_a.reshape(B, N, C, H, dk), axis=2)
    qg = qf * jnp.exp(b)
    kg = kf * jnp.exp(-b)
    causal = jnp.tril(jnp.ones((C, C), dtype=bool))
    A = jnp.where(causal, jnp.einsum('bnihd,bnjhd->bnhij', qg, kg), 0.0)
    o_intra = jnp.einsum('bnhij,bnjhv->bnihv', A, vf)
    b_last = b[:, :, -1]
    kd = kf * jnp.exp(b_last[:, :, None] - b)
    kv = jnp.einsum('bnjhd,bnjhv->bnhdv', kd, vf)
    decay = jnp.exp(b_last)

    def step(S, inp):
        dec, kvn = inp
        return dec[..., None] * S + kvn, S

    S0 = jnp.zeros((B, H, dk, dv), jnp.float32)
    _, S_prev = lax.scan(step, S0, (decay.transpose(1, 0, 2, 3), kv.transpose(1, 0, 2, 3, 4)))
    S_prev = S_prev.transpose(1, 0, 2, 3, 4)
    o_inter = jnp.einsum('bnihd,bnhdv->bnihv', qg, S_prev)
    return (o_intra + o_inter).reshape(B, T, H, dv)


def hier_moe(x, w_gr, b_gr, w_er, b_er, w_e_in, w_e_out):
    B, T, D = x.shape
    xt = x.reshape(-1, D)
    Ntok = xt.shape[0]
    glog = (xt @ w_gr + b_gr).astype(jnp.float32)
    gsel = jnp.argmax(glog, axis=-1)
    pg = jnp.take_along_axis(jax.nn.softmax(glog, axis=-1), gsel[:, None], axis=-1)
    elog = (xt @ w_er + b_er).astype(jnp.float32).reshape(Ntok, N_GROUPS, EXPERTS_PER_GROUP)
    elog_g = jnp.take_along_axis(elog, gsel[:, None, None], axis=1)[:, 0]
    tv, te = lax.top_k(elog_g, TOP_K_INNER)
    gate = jax.nn.softmax(tv, axis=-1) * pg
    eid = (gsel[:, None] * EXPERTS_PER_GROUP + te).reshape(-1)
    tok = jnp.repeat(jnp.arange(Ntok), TOP_K_INNER)
    order = jnp.argsort(eid)
    tok_s = tok[order]
    gate_s = gate.reshape(-1)[order]
    sizes = jnp.bincount(eid, length=N_EXPERTS).astype(jnp.int32)
    xs = xt[tok_s]
    h = lax.ragged_dot(xs, w_e_in, sizes)
    hg, hu = jnp.split(h, 2, axis=-1)
    y = lax.ragged_dot(jax.nn.silu(hg) * hu, w_e_out, sizes)
    out = jnp.zeros_like(xt).at[tok_s].add(y * gate_s[:, None].astype(y.dtype))
    return out.reshape(B, T, D)


def setup_inputs(seed: int = 0) -> dict:
    key = jax.random.key(seed)
    ks = jax.random.split(key, 16)

    def nrm(k, shape, scale):
        return jax.random.normal(k, shape, jnp.float32) * scale

    return {
        'x': nrm(ks[0], (BATCH, SEQ, D_MODEL), 1.0),
        'w_in': nrm(ks[1], (DEPTH, D_MODEL, IN_WIDTH), D_MODEL ** -0.5),
        'w_gla_gate': nrm(ks[2], (DEPTH, GLA_GATE_RANK, GLA_HEADS * GLA_DK), GLA_GATE_RANK ** -0.5),
        'b_gla_gate': nrm(ks[3], (DEPTH, GLA_HEADS * GLA_DK), 0.1),
        'g_gla_norm': 1.0 + nrm(ks[4], (DEPTH, GLA_DV), 0.02),
        'w_out': nrm(ks[5], (DEPTH, D_MODEL, D_MODEL), D_MODEL ** -0.5 * DEEPNORM_BETA),
        'ln1_g': 1.0 + nrm(ks[6], (DEPTH, D_MODEL), 0.02),
        'ln1_b': nrm(ks[7], (DEPTH, D_MODEL), 0.02),
        'w_group_router': nrm(ks[8], (DEPTH, D_MODEL, N_GROUPS), D_MODEL ** -0.5),
        'b_group_router': nrm(ks[9], (DEPTH, N_GROUPS), 0.01),
        'w_expert_router': nrm(ks[10], (DEPTH, D_MODEL, N_EXPERTS), D_MODEL ** -0.5),
        'b_expert_router': nrm(ks[11], (DEPTH, N_EXPERTS), 0.01),
        'w_expert_in': nrm(ks[12], (DEPTH, N_EXPERTS, D_MODEL, 2 * D_EXPERT), D_MODEL ** -0.5),
        'w_expert_out': nrm(ks[13], (DEPTH, N_EXPERTS, D_EXPERT, D_MODEL), D_EXPERT ** -0.5 * DEEPNORM_BETA),
        'ln2_g': 1.0 + nrm(ks[14], (DEPTH, D_MODEL), 0.02),
        'ln2_b': nrm(ks[15], (DEPTH, D_MODEL), 0.02),
    }


def reference(x, w_in, w_gla_gate, b_gla_gate, g_gla_norm, w_out, ln1_g, ln1_b,
              w_group_router, b_group_router, w_expert_router, b_expert_router,
              w_expert_in, w_expert_out, ln2_g, ln2_b):
    B, T, D = x.shape
    pos = jnp.arange(T)
    split_points = np.cumsum(np.array(IN_SIZES))[:-1].tolist()
    h = x
    for l in range(DEPTH):
        proj = h @ w_in[l]
        aq, ak, av, iq, ik, iw, bq, bk, bv, br, bg = jnp.split(proj, split_points, axis=-1)
        aq = rope(aq.reshape(B, T, DSA_HEADS, DSA_HEAD_DIM), pos)
        ak = rope(ak.reshape(B, T, DSA_HEADS, DSA_HEAD_DIM), pos)
        av = av.reshape(B, T, DSA_HEADS, DSA_HEAD_DIM)
        iq = rope(iq.reshape(B, T, IDX_HEADS, IDX_DIM), pos)
        ik = rope(ik[:, :, None, :], pos)[:, :, 0]
        iw = iw * IDX_SCALE
        ya = dsa_mixer(aq, ak, av, iq, ik, iw)
        log_a = jax.nn.log_sigmoid((bg @ w_gla_gate[l] + b_gla_gate[l]).astype(jnp.float32)) / GLA_TAU
        ob = gla_mixer(bq.reshape(B, T, GLA_HEADS, GLA_DK), bk.reshape(B, T, GLA_HEADS, GLA_DK),
                       bv.reshape(B, T, GLA_HEADS, GLA_DV), log_a.reshape(B, T, GLA_HEADS, GLA_DK))
        ob = ob * lax.rsqrt(jnp.mean(jnp.square(ob), axis=-1, keepdims=True) + LN_EPS) * g_gla_norm[l]
        yb = (ob.reshape(B, T, GLA_WIDTH) * jax.nn.silu(br.astype(jnp.float32))).astype(h.dtype)
        mix = jnp.concatenate([ya, yb], axis=-1) @ w_out[l]
        h = layer_norm(DEEPNORM_ALPHA * h + mix, ln1_g[l], ln1_b[l])
        ffn = hier_moe(h, w_group_router[l], b_group_router[l], w_expert_router[l], b_expert_router[l],
                       w_expert_in[l], w_expert_out[l])
        h = layer_norm(DEEPNORM_ALPHA * h + ffn, ln2_g[l], ln2_b[l])
    return h
```

```python
import os
import numpy as np
from contextlib import ExitStack
import concourse.bass as bass
import concourse.mybir as mybir
from concourse.bass_utils import run_bass_kernel_spmd

F32 = mybir.dt.float32
BF16 = mybir.dt.bfloat16
AF = mybir.ActivationFunctionType
ALU = mybir.AluOpType

NSLOT = 64
T = 8192
TO = 4096
NEG = -30000.0
EPS = 1e-5
ALPHA = 2.0 ** 0.25
NIT = 24


class Res:
    __slots__ = ("w", "r")

    def __init__(self):
        self.w = None
        self.r = []


class Op:
    __slots__ = ("eng", "fn", "deps", "idx", "signal", "sem", "val", "dma", "presem")

    def __init__(self, eng, fn, dma):
        self.eng = eng
        self.fn = fn
        self.dma = dma
        self.deps = []
        self.signal = False
        self.sem = None
        self.val = 0
        self.presem = None


class Prog:
    ENGS = ("pe", "act", "dve", "pool", "sp")
    CHUNK = 30000
    NDMASEM = int(os.environ.get("NDMASEM", "8"))

    def __init__(self, nc):
        self.nc = nc
        self.ops = {e: [] for e in self.ENGS}
        self.outs = []
        self.dmas = []

    def add(self, eng, fn, reads=(), writes=(), dma=False, out=False):
        op = Op(eng, fn, dma)
        deps = {}
        for r in reads:
            if r.w is not None:
                deps[id(r.w)] = r.w
        for w in writes:
            if w.w is not None:
                deps[id(w.w)] = w.w
            for rr in w.r:
                deps[id(rr)] = rr
        best = {}
        lst = []
        for d in deps.values():
            if d.dma:
                lst.append(d)
            else:
                if d.eng == "pe" and eng == "pe" and not dma:
                    continue
                b = best.get(d.eng)
                if b is None or d.idx > b.idx:
                    best[d.eng] = d
        lst.extend(best.values())
        op.deps = lst
        for d in lst:
            d.signal = True
        op.idx = len(self.ops[eng])
        self.ops[eng].append(op)
        for r in reads:
            r.r.append(op)
        for w in writes:
            w.w = op
            w.r = []
        if dma:
            self.dmas.append(op)
        if out:
            op.signal = True
            self.outs.append(op)
        return op

    def barrier(self):
        lasts = []
        for e in self.ENGS:
            for op in reversed(self.ops[e]):
                if not op.dma and op.fn is not None:
                    op.signal = True
                    lasts.append(op)
                    break
        for d in self.dmas:
            d.signal = True
        deps = lasts + self.dmas
        self.dmas = []
        for e in self.ENGS:
            op = Op(e, None, False)
            op.deps = list(deps)
            op.idx = len(self.ops[e])
            self.ops[e].append(op)

    def emit(self, es):
        nc = self.nc
        for e in self.ENGS:
            cnt = 0
            sem = None
            k = 0
            dsems = []
            dcnt = []
            di = 0
            for op in self.ops[e]:
                if not op.signal:
                    continue
                if op.dma:
                    j = di % self.NDMASEM
                    di += 1
                    if j >= len(dsems):
                        dsems.append(es.enter_context(nc.semaphore(f"d_{e}_{len(dsems)}")))
                        dcnt.append(0)
                    op.sem = dsems[j]
                    if dcnt[j] > 0:
                        op.presem = (dsems[j], 16 * dcnt[j])
                    dcnt[j] += 1
                    op.val = 16 * dcnt[j]
                else:
                    if sem is None or cnt >= self.CHUNK:
                        sem = es.enter_context(nc.semaphore(f"c_{e}_{k}"))
                        k += 1
                        cnt = 0
                    cnt += 1
                    op.sem = sem
                    op.val = cnt
        fin = Op("sp", None, False)
        fin.deps = list(self.outs)
        self.ops["sp"].append(fin)
        block = es.enter_context(nc.Block())
        prog = self

        def run(e, eng):
            waited = {}
            for op in prog.ops[e]:
                need = {}
                for d in op.deps:
                    key = id(d.sem)
                    if need.get(key, (None, 0))[1] < d.val:
                        need[key] = (d.sem, d.val)
                if op.presem is not None:
                    key = id(op.presem[0])
                    if need.get(key, (None, 0))[1] < op.presem[1]:
                        need[key] = op.presem
                for key, (s, v) in need.items():
                    if waited.get(key, 0) >= v:
                        continue
                    waited[key] = v
                    eng.wait_ge(s, v)
                if op.fn is None:
                    continue
                ins = op.fn(eng)
                if op.signal:
                    ins.then_inc(op.sem, 16 if op.dma else 1)

        @block.tensor
        def _(eng):
            run("pe", eng)

        @block.scalar
        def _(eng):
            run("act", eng)

        @block.vector
        def _(eng):
            run("dve", eng)

        @block.gpsimd
        def _(eng):
            run("pool", eng)

        @block.sync
        def _(eng):
            run("sp", eng)


class Buf:
    def __init__(self, ap, tracked=True):
        self.ap = ap
        self.res = Res()
        self.tracked = tracked

    def __getitem__(self, k):
        return self.ap[k]


def _rl(bs):
    return [b.res for b in bs if b.tracked]


class Ring:
    def __init__(self, bufs):
        self.bufs = bufs
        self.i = 0

    def nxt(self):
        b = self.bufs[self.i % len(self.bufs)]
        self.i += 1
        return b


class K:
    def __init__(self, P):
        self.P = P

    def mm(self, out, lhsT, rhs, start, stop, R, W):
        self.P.add("pe", lambda e: e.matmul(out, lhsT=lhsT, rhs=rhs, start=start, stop=stop),
                   _rl(R), _rl(W))

    def tr(self, out, in_, ident, R, W):
        self.P.add("pe", lambda e: e.transpose(out=out, in_=in_, identity=ident),
                   _rl(R), _rl(W))

    def act(self, out, in_, func, R, W, scale=1.0, bias=None, accum=None):
        kw = {}
        if bias is not None:
            kw["bias"] = bias
        if accum is not None:
            kw["accum_out"] = accum
        self.P.add("act", lambda e: e.activation(out=out, in_=in_, func=func, scale=scale, **kw),
                   _rl(R), _rl(W))

    def tt(self, eng, out, in0, in1, op, R, W):
        self.P.add(eng, lambda e: e.tensor_tensor(out=out, in0=in0, in1=in1, op=op),
                   _rl(R), _rl(W))

    def ts(self, eng, out, in0, s1, s2, op0, op1, R, W, accum=None):
        kw = {}
        if accum is not None:
            kw["accum_out"] = accum
        if op1 is None:
            self.P.add(eng, lambda e: e.tensor_scalar(out=out, in0=in0, scalar1=s1, scalar2=None, op0=op0, **kw),
                       _rl(R), _rl(W))
        else:
            self.P.add(eng, lambda e: e.tensor_scalar(out=out, in0=in0, scalar1=s1, scalar2=s2, op0=op0, op1=op1, **kw),
                       _rl(R), _rl(W))

    def stt(self, out, in0, scalar, in1, op0, op1, R, W):
        self.P.add("dve", lambda e: e.scalar_tensor_tensor(out=out, in0=in0, scalar=scalar, in1=in1, op0=op0, op1=op1),
                   _rl(R), _rl(W))

    def cp(self, eng, out, in_, R, W):
        if eng == "act":
            self.P.add("act", lambda e: e.copy(out=out, in_=in_), _rl(R), _rl(W))
        else:
            self.P.add(eng, lambda e: e.tensor_copy(out=out, in_=in_), _rl(R), _rl(W))

    def memset(self, eng, ap, val, W):
        self.P.add(eng, lambda e: e.memset(ap, val), [], _rl(W))

    def dma(self, out, in_, R, W, final=False):
        self.P.add("sp", lambda e: e.dma_start(out=out, in_=in_), _rl(R), _rl(W),
                   dma=True, out=final)

    def reduce(self, out, in_, op, R, W):
        self.P.add("dve", lambda e: e.tensor_reduce(out=out, in_=in_, axis=mybir.AxisListType.X, op=op), _rl(R), _rl(W))

    def gather(self, out, in_, idx, R, W, bound=None):
        regs = self.__dict__.setdefault("_bregs", {})

        def fn(e):
            if bound is None:
                return e.indirect_dma_start(out=out, out_offset=None, in_=in_,
                                            in_offset=bass.IndirectOffsetOnAxis(ap=idx, axis=0))
            if bound not in regs:
                regs[bound] = e.to_reg(bound)
            return e.indirect_dma_start(out=out, out_offset=None, in_=in_,
                                        in_offset=bass.IndirectOffsetOnAxis(ap=idx, axis=0),
                                        bounds_check=regs[bound], oob_is_err=False)

        self.P.add("pool", fn, _rl(R), _rl(W), dma=True)

    def scatter(self, out, in_, idx, bound, R, W):
        regs = self.__dict__.setdefault("_bregs", {})

        def fn(e):
            if bound not in regs:
                regs[bound] = e.to_reg(bound)
            return e.indirect_dma_start(out=out, out_offset=bass.IndirectOffsetOnAxis(ap=idx, axis=0),
                                        in_=in_, in_offset=None, bounds_check=regs[bound], oob_is_err=False)

        self.P.add("pool", fn, _rl(R), _rl(W), dma=True)

    def scan(self, out, d0, d1, init, op0, op1, R, W):
        self.P.add("dve", lambda e: e.tensor_tensor_scan(out=out, data0=d0, data1=d1, initial=init, op0=op0, op1=op1),
                   _rl(R), _rl(W))

    def recip(self, out, in_, R, W):
        self.P.add("dve", lambda e: e.reciprocal(out=out, in_=in_), _rl(R), _rl(W))

    def bn_stats(self, out, in_, R, W):
        self.P.add("dve", lambda e: e.bn_stats(out=out, in_=in_), _rl(R), _rl(W))

    def bn_aggr(self, out, in_, R, W):
        self.P.add("dve", lambda e: e.bn_aggr(out=out, in_=in_), _rl(R), _rl(W))


class Arena:
    def __init__(self, ap, cols):
        self.ap = ap
        self.cols = cols
        self.off = 0

    def reset(self):
        self.off = 0

    def alloc(self, cols, dtype=BF16, parts=128):
        n = cols * (1 if dtype == BF16 else 2)
        n = (n + 1) // 2 * 2
        assert self.off + n <= self.cols, (self.off, n, self.cols)
        v = self.ap[0:parts, self.off:self.off + n]
        self.off += n
        if dtype != BF16:
            v = v.bitcast(dtype)
        return Buf(v)

    def ring(self, k, cols, dtype=BF16, parts=128):
        return Ring([self.alloc(cols, dtype, parts) for _ in range(k)])


import os
SKIP = set(os.environ.get('KSKIP', '').split(','))
NFK = 13
NTS = 96
MOE_SORTED = os.environ.get('MOE', 'sorted') == 'sorted'
NFQ = 19
DBG = {}


def build(stage=99, nslot=NSLOT, nexp=32):
    nc = bass.Bass("TRN2", target_bir_lowering=False)

    def din(name, shape, dt=F32):
        return Buf(nc.dram_tensor(name, shape, dt, kind="ExternalInput").ap(), tracked=False)

    def dscr(name, shape, dt=BF16, dbg=False):
        kind = "ExternalOutput" if dbg else "Internal"
        return Buf(nc.dram_tensor(name, shape, dt, kind=kind).ap(), tracked=False)

    d_xT = din("xT", [1024, T])
    d_xo = din("xo", [TO, 1024])
    d_ca = din("ca", [128, T])
    d_sa = din("sa", [128, T])
    d_ci = din("ci", [128, T])
    d_si = din("si", [128, T])
    d_wfk = din("wfk", [1024, NFK * 128])
    d_wfq = din("wfq", [1024, NFQ * 128])
    d_wtk = din("wtk", [1024, 1280])
    d_wtq = din("wtq", [1024, 520])
    d_wg = din("wg", [17, 256])
    d_gn = din("gn", [1, 128])
    d_wout = din("wout", [1024, 1024])
    d_ln1g = din("ln1g", [1, 1024])
    d_ln1b = din("ln1b", [1, 1024])
    d_wr = din("wr", [1024, 36])
    d_brr = din("brr", [1, 36])
    d_wein = din("wein", [32, 1024, 1024])
    d_weout = din("weout", [32, 512, 1024])
    d_ln2g = din("ln2g", [1, 1024])
    d_ln2b = din("ln2b", [1, 1024])
    d_dummyb = din("dummyb", [128, 128])
    d_cmask = din("cmask", [128, 6 * 128])
    d_out = Buf(nc.dram_tensor("out", [TO, 1024], F32, kind="ExternalOutput").ap(), tracked=False)
    d_rc = din("rconst", [128, 2 * 128 + 2048 + 96 + 1 + 32])

    dbg = stage < 99
    s_KT = dscr("s_KT", [128, NSLOT, 4, 128], BF16, dbg and stage == 1)
    s_IKT = dscr("s_IKT", [96, T], BF16, dbg and stage == 1)
    s_V = dscr("s_V", [T, 520], BF16, dbg and stage == 1)
    s_QT = dscr("s_QT", [128, 32, 4, 128], BF16, dbg and stage == 1)
    s_IQT = dscr("s_IQT", [96, 32, 3, 128], BF16, dbg and stage == 1)
    s_IW = dscr("s_IW", [TO, 8], F32, dbg and stage == 1)
    s_KGT = dscr("s_KGT", [128, NSLOT, 2, 128], BF16, dbg and stage == 1)
    s_QGT = dscr("s_QGT", [128, 32, 2, 128], BF16, dbg and stage == 1)
    s_KD = dscr("s_KD", [T, 256], BF16, dbg and stage == 1)
    s_VG = dscr("s_VG", [T, 512], BF16, dbg and stage == 1)
    s_DEC = dscr("s_DEC", [128, 2, 128], F32, dbg and stage == 1)
    s_GS = dscr("s_GS", [TO, 512], F32, dbg and stage == 1)
    s_YT = dscr("s_YT", [128, 32, 8, 128], BF16, dbg and stage in (2, 3))
    s_THR = dscr("s_THR", [TO, 2], F32, dbg and stage == 3)
    s_H1 = dscr("s_H1", [TO, 1024], F32, dbg and stage == 4)
    s_H1T = dscr("s_H1T", [128, 8, TO], BF16, dbg and stage == 4)
    s_GATE = dscr("s_GATE", [TO, 32], F32, dbg and stage == 4)
    I32 = mybir.dt.int32
    s_H1B = dscr("s_H1B", [TO, 1024], BF16)
    s_WBI = dscr("s_WBI", [32 * 128, 8192], BF16)
    s_WBO = dscr("s_WBO", [32 * 128, 4096], BF16)
    s_TAB = Buf(nc.dram_tensor("s_TAB", [NTS * 128, 16], I32, kind="Internal").ap())
    s_WIDX = dscr("s_WIDX", [128, NTS], I32)
    s_Y2 = dscr("s_Y2", [2 * TO, 1024], F32)

    es = ExitStack()
    with es:
        P = Prog(nc)
        k = K(P)
        arena_t = es.enter_context(nc.sbuf_tensor("arena", [128, 104000], BF16))
        A = Arena(arena_t, 104000)
        banks_t = [es.enter_context(nc.psum_tensor(f"bank{i}", [128, 512], F32)) for i in range(8)]
        tab_t = [es.enter_context(nc.sbuf_tensor(f"tabt{i}", [128, 16], mybir.dt.int32)) for i in range(3)]

        def banks():
            return [Buf(b[:, :]) for b in banks_t]

        MUL, ADD, SUB = ALU.mult, ALU.add, ALU.subtract

        PB = banks()
        cm = A.alloc(6 * 128, F32)
        k.dma(cm[:, :], d_cmask[:, :], [d_cmask], [cm])
        ident_f = cm[:, 0:128]
        triinc = cm[:, 128:256]
        trirev = cm[:, 256:384]
        wfk = A.alloc(8 * NFK * 128)
        wfq = A.alloc(8 * NFQ * 128)
        wtk = A.alloc(8 * 1280)
        wtq = A.alloc(8 * 520)
        wfk_v = wfk.ap.rearrange("p (c f) -> p c f", c=8)
        wfq_v = wfq.ap.rearrange("p (c f) -> p c f", c=8)
        wtk_v = wtk.ap.rearrange("p (c f) -> p c f", c=8)
        wtq_v = wtq.ap.rearrange("p (c f) -> p c f", c=8)
        wg = A.alloc(256, BF16, 32)
        wgs = A.alloc(256, F32, 32)
        stg = A.ring(2, 1024, F32)
        cvt_i = [0]

        def load_w(dram, view, ncols, dst):
            for c in range(8):
                for c0 in range(0, ncols, 1024):
                    w_ = min(1024, ncols - c0)
                    s = stg.nxt()
                    k.dma(s[:, 0:w_], dram[c * 128:(c + 1) * 128, c0:c0 + w_], [dram], [s])
                    eng = "act" if cvt_i[0] % 2 == 0 else "dve"
                    cvt_i[0] += 1
                    k.cp(eng, view[:, c, c0:c0 + w_], s[:, 0:w_], [s], [dst])

        load_w(d_wfk, wfk_v, NFK * 128, wfk)
        load_w(d_wfq, wfq_v, NFQ * 128, wfq)
        load_w(d_wtk, wtk_v, 1280, wtk)
        load_w(d_wtq, wtq_v, 520, wtq)
        k.dma(wgs[0:17, :], d_wg[:, :], [d_wg], [wgs])
        k.cp("dve", wg[0:17, :], wgs[0:17, :], [wgs], [wg])
        gbc = A.alloc(512, F32)
        for h in range(4):
            k.dma(gbc[:, h * 128:(h + 1) * 128], d_gn.ap.partition_broadcast(128), [d_gn], [gbc])

        xs = A.alloc(8 * 512, F32)
        xb = A.ring(2, 8 * 512)
        tabs = [A.alloc(512, F32) for _ in range(4)]
        t1r = A.ring(2, 512, F32)
        t2r = A.ring(2, 512, F32)
        obr = A.ring(2, 512)
        kt4r = A.ring(2, 2048)
        qt4r = A.ring(2, 1024)
        iq3r = A.ring(2, 768)
        bkT = A.alloc(2 * 512, F32)
        bgT = A.alloc(512)
        k.memset("pool", bgT[0:32, :], 1.0, [bgT])
        decs = A.alloc(2 * 128, F32)
        decs_v = decs.ap.rearrange("p (f n) -> p f n", f=2)
        k.memset("pool", decs[:, :], 1.0, [decs])
        tz = A.alloc(256, F32)
        la = A.alloc(256, F32)
        erev = A.alloc(256, F32)
        e1 = A.alloc(256, F32)
        e2 = A.alloc(256, F32)
        vout = A.ring(2, 520)
        for b_ in vout.bufs:
            k.memset("pool", b_[:, :], 1.0, [b_])
        vgout = A.ring(2, 512)
        kdout = A.ring(2, 256)
        kgout = A.ring(2, 256)
        qgout = A.ring(2, 256)
        gsout = A.ring(2, 512, F32)
        sil = A.alloc(512, F32)
        iwout = A.ring(2, 8, F32)

        xTv = d_xT.ap.rearrange("(c p) t -> p c t", p=128)
        nblk = nslot // 4
        pi = [0]

        def feat_tile(wview, wbuf, ti, xbuf, xcols, ncol, bank, col0, M=128):
            xv = xbuf.ap.rearrange("p (c t) -> p c t", c=8)
            for c in range(8):
                k.mm(bank[0:M, col0:col0 + ncol], wview[:, c, ti * 128:ti * 128 + M], xv[:, c, xcols[0]:xcols[1]],
                     c == 0, c == 7, [wbuf, xbuf], [bank])

        def rope_out(bP, bR, ncol, tc_, ts_, rows, scale, ob_ap, obuf):
            t1 = t1r.nxt()
            t2 = t2r.nxt()
            nseg = ncol // 128
            c_ap, s_ap = tc_[0:rows, 0:ncol], ts_[0:rows, 0:ncol]
            k.stt(t1[0:rows, 0:ncol], bP[0:rows, 0:ncol], scale, c_ap, MUL, MUL, [bP, tc_], [t1])
            k.stt(t2[0:rows, 0:ncol], bR[0:rows, 0:ncol], scale, s_ap, MUL, MUL, [bR, ts_], [t2])
            v = lambda b_: b_.ap[0:rows, 0:ncol].rearrange("p (s t) -> p s t", s=nseg)
            k.tt("pool", ob_ap, v(t1), v(t2), ADD, [t1, t2], [obuf])

        for B in range(nblk):
            c0 = B * 512
            for c in range(8):
                k.dma(xs[:, c * 512:(c + 1) * 512], d_xT[c * 128:(c + 1) * 128, c0:c0 + 512], [d_xT], [xs])
            for tb, dt_ in zip(tabs, (d_ca, d_sa, d_ci, d_si)):
                k.dma(tb[:, :], dt_[:, c0:c0 + 512], [dt_], [tb])
            xbb = xb.nxt()
            k.cp("act", xbb[:, :], xs[:, :], [xs], [xbb])
            kt4 = kt4r.nxt()
            kt4_v = kt4.ap.rearrange("p (s f t) -> p s f t", s=4, f=4)
            for i in range(4):
                bP, bR = PB[(pi[0] * 2) % 4], PB[(pi[0] * 2 + 1) % 4]
                pi[0] += 1
                feat_tile(wfk_v, wfk, i, xbb, (0, 512), 512, bP, 0)
                feat_tile(wfk_v, wfk, 4 + i, xbb, (0, 512), 512, bR, 0)
                rope_out(bP, bR, 512, tabs[0], tabs[1], 128, 1.0, kt4_v[:, :, i, :], kt4)
            k.dma(s_KT[:, 4 * B:4 * B + 4].rearrange("p s f t -> p (s f t)"), kt4[:, :], [kt4], [])
            bP, bR = PB[(pi[0] * 2) % 4], PB[(pi[0] * 2 + 1) % 4]
            pi[0] += 1
            feat_tile(wfk_v, wfk, 8, xbb, (0, 512), 512, bP, 0, M=96)
            feat_tile(wfk_v, wfk, 9, xbb, (0, 512), 512, bR, 0, M=96)
            ob = obr.nxt()
            rope_out(bP, bR, 512, tabs[2], tabs[3], 96, 1.0, ob.ap[0:96, 0:512].rearrange("p (s t) -> p s t", s=4), ob)
            k.dma(s_IKT[:, c0:c0 + 512], ob[0:96, 0:512], [ob], [])
            for ft in range(2):
                bP = PB[(pi[0] * 2) % 4]
                pi[0] += 1
                feat_tile(wfk_v, wfk, 10 + ft, xbb, (0, 512), 512, bP, 0)
                k.cp("act", bkT[:, ft * 512:(ft + 1) * 512], bP[:, :], [bP], [bkT])
            bP = PB[(pi[0] * 2) % 4]
            pi[0] += 1
            feat_tile(wfk_v, wfk, 12, xbb, (0, 512), 512, bP, 0, M=16)
            k.cp("act", bgT[0:16, :], bP[0:16, :], [bP], [bgT])
            own_cols = [(128, 256), (384, 512)] if 'q' not in SKIP else []

            def tv_own(tb, rows):
                return tb.ap.rearrange("p (s t) -> p s t", s=4)[0:rows, 1::2, :]

            oc0 = B * 256
            qt4 = qt4r.nxt()
            qt4_v = qt4.ap.rearrange("p (u f t) -> p u f t", u=2, f=4)
            iq3 = iq3r.nxt()
            iq3_v = iq3.ap.rearrange("p (u g t) -> p u g t", u=2, g=3)
            for i in range(4 if 'q' not in SKIP else 0):
                bP, bR = PB[(pi[0] * 2) % 4], PB[(pi[0] * 2 + 1) % 4]
                pi[0] += 1
                for u, xc in enumerate(own_cols):
                    feat_tile(wfq_v, wfq, i, xbb, xc, 128, bP, u * 128)
                    feat_tile(wfq_v, wfq, 4 + i, xbb, xc, 128, bR, u * 128)
                t1 = t1r.nxt()
                t2 = t2r.nxt()
                v3 = lambda b_, rows: b_.ap[0:rows, 0:256].rearrange("p (s t) -> p s t", s=2)
                k.stt(v3(t1, 128), v3(bP, 128), 0.125, tv_own(tabs[0], 128), MUL, MUL, [bP, tabs[0]], [t1])
                k.stt(v3(t2, 128), v3(bR, 128), 0.125, tv_own(tabs[1], 128), MUL, MUL, [bR, tabs[1]], [t2])
                k.tt("pool", qt4_v[:, :, i, :], v3(t1, 128), v3(t2, 128), ADD, [t1, t2], [qt4])
            if 'q' not in SKIP:
                k.dma(s_QT[:, 2 * B:2 * B + 2].rearrange("p u f t -> p (u f t)"), qt4[:, :], [qt4], [])
            for i in range(3 if 'q' not in SKIP else 0):
                bP, bR = PB[(pi[0] * 2) % 4], PB[(pi[0] * 2 + 1) % 4]
                pi[0] += 1
                for u, xc in enumerate(own_cols):
                    feat_tile(wfq_v, wfq, 8 + i, xbb, xc, 128, bP, u * 128, M=96)
                    feat_tile(wfq_v, wfq, 11 + i, xbb, xc, 128, bR, u * 128, M=96)
                t1 = t1r.nxt()
                t2 = t2r.nxt()
                k.stt(v3(t1, 96), v3(bP, 96), 1.0, tv_own(tabs[2], 96), MUL, MUL, [bP, tabs[2]], [t1])
                k.stt(v3(t2, 96), v3(bR, 96), 1.0, tv_own(tabs[3], 96), MUL, MUL, [bR, tabs[3]], [t2])
                k.tt("pool", v3(t1, 96), v3(t1, 96), v3(t2, 96), ADD, [t1, t2], [t1])
                bA = PB[(pi[0] * 2) % 4]
                pi[0] += 1
                for u, xc in enumerate(own_cols):
                    feat_tile(wfq_v, wfq, 16 + i, xbb, xc, 128, bA, u * 128, M=96)
                t3 = t2r.nxt()
                k.act(t3[0:96, 0:256], bA[0:96, 0:256], AF.Abs, [bA], [t3], scale=1.0 / 16.0)
                k.tt("pool", iq3_v[0:96, :, i, :], v3(t1, 96), v3(t3, 96), MUL, [t1, t3], [iq3])
            if 'q' not in SKIP:
                k.dma(s_IQT[:, 2 * B:2 * B + 2].rearrange("p u g t -> p (u g t)"), iq3[0:96, :], [iq3], [])
            xv = xbb.ap.rearrange("p (c t) -> p c t", c=8)
            for sl in range(4 if 'slot' not in SKIP else 0):
                s = B * 4 + sl
                cs = sl * 128
                r0 = s * 128
                bG, bH = PB[4], PB[5]
                k.mm(bG[:, 0:256], bgT[0:17, cs:cs + 128], wg[0:17, :], True, True, [bgT, wg], [bG])
                k.act(tz[:, :], bG[:, 0:256], AF.Exp, [bG], [tz], scale=-1.0)
                k.act(la[:, :], tz[:, :], AF.Ln, [tz], [la], bias=1.0)
                k.mm(bG[:, 256:512], trirev, la[:, :], True, True, [cm, la], [bG])
                k.act(erev[:, :], bG[:, 256:512], AF.Exp, [bG], [erev])
                for ft in range(2):
                    k.mm(bH[:, ft * 128:(ft + 1) * 128], la[:, ft * 128:(ft + 1) * 128], triinc, True, True, [la, cm], [bH])
                k.act(e1[:, :], bH[:, 0:256], AF.Exp, [bH], [e1])
                k.act(e2[:, :], bH[:, 0:256], AF.Exp, [bH], [e2], scale=-1.0)
                e1v = e1.ap.rearrange("p (f t) -> p f t", f=2)
                k.cp("pool", decs_v[:, :, 2 * s:2 * s + 2], e1v[:, :, 63::64], [e1], [decs])
                kg = kgout.nxt()
                bkv = bkT.ap.rearrange("p (f t) -> p f t", f=2)[:, :, cs:cs + 128]
                k.tt("dve", kg.ap.rearrange("p (f t) -> p f t", f=2), bkv, e2.ap.rearrange("p (f t) -> p f t", f=2),
                     MUL, [bkT, e2], [kg])
                k.dma(s_KGT[:, s].rearrange("p f t -> p (f t)"), kg[:, :], [kg], [])
                bV, bW, bK = PB[6], PB[7], PB[5]
                for c in range(8):
                    k.mm(bV[:, :], xv[:, c, cs:cs + 128], wtk_v[:, c, 0:512], c == 0, c == 7, [xbb, wtk], [bV])
                vo = vout.nxt()
                k.cp("act", vo.ap.rearrange("p (h d) -> p h d", h=8)[:, :, 0:64],
                     bV.ap.rearrange("p (h d) -> p h d", h=8), [bV], [vo])
                k.dma(s_V[r0:r0 + 128, :], vo[:, :], [vo], [s_V])
                for c in range(8):
                    k.mm(bW[:, :], xv[:, c, cs:cs + 128], wtk_v[:, c, 512:1024], c == 0, c == 7, [xbb, wtk], [bW])
                vg = vgout.nxt()
                k.cp("dve", vg[:, :], bW[:, :], [bW], [vg])
                k.dma(s_VG[r0:r0 + 128, :], vg[:, :], [vg], [s_VG])
                for c in range(8):
                    k.mm(bK[:, 256:512], xv[:, c, cs:cs + 128], wtk_v[:, c, 1024:1280], c == 0, c == 7, [xbb, wtk], [bK])
                kd = kdout.nxt()
                k.tt("dve", kd[:, :], bK[:, 256:512], erev[:, :], MUL, [bK, erev], [kd])
                k.dma(s_KD[r0:r0 + 128, :], kd[:, :], [kd], [s_KD])
                if sl % 2 == 1:
                    j = s // 2
                    o0 = j * 128
                    bQ = PB[6]
                    for ft in range(2):
                        feat_tile(wfq_v, wfq, 14 + ft, xbb, (cs, cs + 128), 128, bQ, ft * 128)
                    qg = qgout.nxt()
                    k.stt(qg[:, :], bQ[:, 0:256], 0.125, e1[:, :], MUL, MUL, [bQ, e1], [qg])
                    k.dma(s_QGT[:, j].rearrange("p f t -> p (f t)"), qg[:, :], [qg], [])
                    bBR = PB[7]
                    for c in range(8):
                        k.mm(bBR[:, :], xv[:, c, cs:cs + 128], wtq_v[:, c, 0:512], c == 0, c == 7, [xbb, wtq], [bBR])
                    k.act(sil[:, :], bBR[:, :], AF.Silu, [bBR], [sil])
                    gs = gsout.nxt()
                    k.tt("pool", gs[:, :], sil[:, :], gbc[:, :], MUL, [sil, gbc], [gs])
                    k.dma(s_GS[o0:o0 + 128, :], gs[:, :], [gs], [s_GS])
                    bIW = PB[6]
                    for c in range(8):
                        k.mm(bIW[:, 256:264], xv[:, c, cs:cs + 128], wtq_v[:, c, 512:520], c == 0, c == 7, [xbb, wtq], [bIW])
                    iw_ = iwout.nxt()
                    k.ts("dve", iw_[:, :], bIW[:, 256:264], 1.0 / 16.0, None, MUL, None, [bIW], [iw_])
                    k.dma(s_IW[o0:o0 + 128, :], iw_[:, :], [iw_], [s_IW])
        k.dma(s_DEC.ap.rearrange("p f n -> p (f n)"), decs[:, :], [decs], [s_DEC])
        P.barrier()
        if stage >= 2:
            build_phase2(nc, P, k, A, banks, nslot, locals())
        wflag = {}
        if stage >= 3:
            g3 = dict(locals())
            build_phase3(nc, P, k, A, banks, nslot, g3)
            wflag["wconv_done"] = g3.get("wconv_done", False)
        if stage >= 4:
            build_phase4(nc, P, k, A, banks, nslot, locals())
        if stage >= 5:
            if MOE_SORTED:
                g5 = dict(locals())
                g5["wconv_done"] = wflag.get("wconv_done", False)
                build_phase5s(nc, P, k, A, banks, nslot, g5)
            else:
                build_phase5(nc, P, k, A, banks, nslot, nexp, locals())
        if stage < 5:
            A.reset()
            z = A.alloc(1024, F32)
            k.memset("pool", z[:, :], 0.0, [z])
            k.dma(d_out[0:128, :], z[:, :], [z], [d_out], final=True)
        for op in P.dmas:
            op.signal = True
            P.outs.append(op)
        P.emit(es)
    return nc


def build_phase2(nc, P, k, A, banks, nslot, g):
    MUL, ADD = ALU.mult, ALU.add
    A.reset()
    PB = banks()
    d_cmask = g["d_cmask"]
    s_KD, s_VG, s_KGT, s_QGT, s_GS, s_DEC, s_YT = (g[n] for n in ("s_KD", "s_VG", "s_KGT", "s_QGT", "s_GS", "s_DEC", "s_YT"))
    cm = A.alloc(768, F32)
    k.dma(cm[:, :], d_cmask[:, :], [], [cm])
    atmask4 = A.alloc(512, F32)
    for h in range(4):
        k.cp("pool", atmask4[:, h * 128:(h + 1) * 128], cm[:, 384:512], [cm], [atmask4])
    identb = A.alloc(128)
    k.cp("dve", identb[:, :], cm[:, 0:128], [cm], [identb])
    dec = A.alloc(256, F32)
    dec_v = dec.ap.rearrange("p (f n) -> p f n", f=2)
    k.dma(dec[:, :], s_DEC.ap.rearrange("p f n -> p (f n)"), [], [dec])
    S = A.alloc(256, F32)
    Sa = A.alloc(256)
    Sb = A.alloc(256)
    qg0 = A.alloc(256)
    qg1 = A.alloc(256)
    for b_ in (S, Sa, Sb, qg0, qg1):
        k.memset("pool", b_[:, :], 0.0, [b_])
    v2 = lambda b_: b_.ap.rearrange("p (f t) -> p f t", f=2)
    S_v, Sa_v, Sb_v, qg0_v, qg1_v = v2(S), v2(Sa), v2(Sb), v2(qg0), v2(qg1)
    kgr = A.ring(2, 256)
    kdr = A.ring(2, 256)
    vgr = A.ring(2, 512)
    qgr = A.ring(2, 256)
    gsr = A.ring(2, 512, F32)
    atm = A.alloc(512)
    yb = A.alloc(512)
    ybT = A.ring(2, 512)
    junk = A.alloc(128, F32)
    ss = A.alloc(4, F32)
    ms = A.alloc(4, F32)
    lnv = A.alloc(4, F32)
    rstd = A.alloc(4, F32)
    bKV, bKV2, bAT, bO, bT, bAT2 = PB[0], PB[1], PB[2], PB[3], PB[4], PB[5]
    bT_bf = bT.ap[:, 0:256].bitcast(BF16)
    for s in range(nslot):
        own = s % 2 == 1
        j = s // 2
        r0 = s * 128
        o0 = j * 128
        kd_ = kdr.nxt()
        k.dma(kd_[:, :], s_KD[r0:r0 + 128, :], [], [kd_])
        vg_ = vgr.nxt()
        k.dma(vg_[:, :], s_VG[r0:r0 + 128, :], [], [vg_])
        if own:
            kg_ = kgr.nxt()
            k.dma(kg_[:, :], s_KGT[:, s].rearrange("p f t -> p (f t)"), [], [kg_])
            qg_ = qgr.nxt()
            k.dma(qg_[:, :], s_QGT[:, j].rearrange("p f t -> p (f t)"), [], [qg_])
            gs_ = gsr.nxt()
            k.dma(gs_[:, :], s_GS[o0:o0 + 128, :], [], [gs_])
        for ft in range(2):
            k.mm(bKV[:, ft * 256:(ft + 1) * 256], kd_[0:64, ft * 128:(ft + 1) * 128], vg_[0:64, ft * 256:(ft + 1) * 256],
                 True, True, [kd_, vg_], [bKV])
        if own:
            for h in range(4):
                ft, pb = h // 2, 64 * (h % 2)
                bnk = bAT if h % 2 == 0 else bAT2
                k.mm(bnk[:, ft * 128:(ft + 1) * 128], v2(kg_)[pb:pb + 64, ft, :], v2(qg_)[pb:pb + 64, ft, :], True, True,
                     [kg_, qg_], [bnk])
            atm_v = atm.ap.rearrange("p (h t) -> p h t", h=4)
            am_v = atmask4.ap.rearrange("p (h t) -> p h t", h=4)[:, 0:2, :]
            for par, bnk in enumerate((bAT, bAT2)):
                k.tt("dve", atm_v[:, par::2, :], bnk.ap[:, 0:256].rearrange("p (h t) -> p h t", h=2), am_v, MUL,
                     [bnk, atmask4], [atm])
            k.cp("pool", qg0_v[:, :, 0:64], v2(qg_)[:, :, 0:64], [qg_], [qg0])
            k.cp("pool", qg1_v[:, :, 64:128], v2(qg_)[:, :, 64:128], [qg_], [qg1])
        for h in range(4):
            ft, pb = h // 2, 64 * (h % 2)
            k.stt(S_v[pb:pb + 64, ft, :], S_v[pb:pb + 64, ft, :], dec_v[pb:pb + 64, ft, 2 * s:2 * s + 1],
                  bKV[pb:pb + 64, ft * 256 + (h % 2) * 128: ft * 256 + (h % 2) * 128 + 128], MUL, ADD, [S, dec, bKV], [S])
        if own:
            k.cp("act", Sb[:, :], S[:, :], [S], [Sb])
        for ft in range(2):
            k.mm(bKV2[:, ft * 256:(ft + 1) * 256], kd_[64:128, ft * 128:(ft + 1) * 128], vg_[64:128, ft * 256:(ft + 1) * 256],
                 True, True, [kd_, vg_], [bKV2])
        if own:
            for h in range(4):
                ft, pb = h // 2, 64 * (h % 2)
                oc = bO[:, h * 128:(h + 1) * 128]
                k.mm(oc, atm[:, h * 128:(h + 1) * 128], vg_[:, h * 128:(h + 1) * 128], True, False, [atm, vg_], [bO])
                k.mm(oc, qg0_v[pb:pb + 64, ft, :], Sa_v[pb:pb + 64, ft, :], False, False, [qg0, Sa], [bO])
                k.mm(oc, qg1_v[pb:pb + 64, ft, :], Sb_v[pb:pb + 64, ft, :], False, True, [qg1, Sb], [bO])
            for h in range(4):
                k.act(junk[:, :], bO[:, h * 128:(h + 1) * 128], AF.Square, [bO], [junk, ss], accum=ss[:, h:h + 1])
            k.ts("dve", ms[:, :], ss[:, :], 1.0 / 128.0, EPS, MUL, ADD, [ss], [ms])
            k.act(lnv[:, :], ms[:, :], AF.Ln, [ms], [lnv])
            k.act(rstd[:, :], lnv[:, :], AF.Exp, [lnv], [rstd], scale=-0.5)
            for h in range(4):
                k.stt(yb[:, h * 128:(h + 1) * 128], bO[:, h * 128:(h + 1) * 128], rstd[:, h:h + 1],
                      gs_[:, h * 128:(h + 1) * 128], MUL, MUL, [bO, rstd, gs_], [yb])
            for h in range(4):
                k.tr(bT_bf[:, h * 128:(h + 1) * 128], yb[:, h * 128:(h + 1) * 128], identb[:, :], [yb, identb], [bT])
            yt = ybT.nxt()
            k.cp("act", yt[:, :], bT_bf[:, 0:512], [bT], [yt])
            k.dma(s_YT[:, j, 4:8, :].rearrange("p f t -> p (f t)"), yt[:, :], [yt], [])
        for h in range(4):
            ft, pb = h // 2, 64 * (h % 2)
            k.stt(S_v[pb:pb + 64, ft, :], S_v[pb:pb + 64, ft, :], dec_v[pb:pb + 64, ft, 2 * s + 1:2 * s + 2],
                  bKV2[pb:pb + 64, ft * 256 + (h % 2) * 128: ft * 256 + (h % 2) * 128 + 128], MUL, ADD, [S, dec, bKV2], [S])
        if not own:
            k.cp("act", Sa[:, :], S[:, :], [S], [Sa])
    P.barrier()


def build_phase3(nc, P, k, A, banks, nslot, g):
    MUL, ADD = ALU.mult, ALU.add
    A.reset()
    PB = banks()
    d_cmask, d_dummyb = g["d_cmask"], g["d_dummyb"]
    s_IKT, s_IQT, s_IW, s_QT, s_KT, s_V, s_YT = (g[n] for n in ("s_IKT", "s_IQT", "s_IW", "s_QT", "s_KT", "s_V", "s_YT"))
    cm = A.alloc(768, F32)
    k.dma(cm[:, :], d_cmask[:, :], [], [cm])
    identf = cm[:, 0:128]
    identb = A.alloc(128)
    k.cp("dve", identb[:, :], cm[:, 0:128], [cm], [identb])
    blockmask = cm[:, 512:640]
    sel64 = cm[:, 640:768]
    dmy = A.alloc(128, F32)
    k.dma(dmy[:, :], d_dummyb[:, :], [], [dmy])
    zl = A.alloc(128)
    k.memset("pool", zl[:, :], 0.0, [zl])
    zr = A.alloc(512)
    k.memset("pool", zr[:, :], 0.0, [zr])
    nkmax = nslot * 128
    ikt = A.alloc(nkmax)
    for c0 in range(0, nkmax, 2048):
        c1 = min(nkmax, c0 + 2048)
        k.dma(ikt[0:96, c0:c1], s_IKT[:, c0:c1], [], [ikt])
    scores = [A.alloc(nkmax, F32), A.alloc(nkmax, F32)]
    junk = A.alloc(nkmax)
    negsel = A.alloc(nkmax)
    nmT = A.alloc(nkmax)
    relu = A.ring(6, 512)
    iqr = A.ring(2, 384)
    iwr = A.ring(2, 8, F32)
    qr = A.ring(3, 1024)
    for b_ in qr.bufs:
        k.memset("pool", b_[:, :], 0.0, [b_])
    yTr = A.ring(2, 512)
    rd = A.alloc(8, F32)
    ktr = A.ring(4, 512)
    vtr = A.ring(4, 520)
    pTr = A.ring(3, 512)
    dgsr = A.ring(2, 8 * 128)
    sgr = A.ring(2, 8, F32)
    mid = A.alloc(2, F32)
    cnt = A.alloc(2, F32)
    sgn = A.alloc(2, F32)
    tq = A.alloc(2, F32)
    tq2 = A.alloc(2, F32)
    thr = A.alloc(2, F32)
    yar = A.ring(2, 512)
    cpi = [0]
    NITER = 15
    W0 = 32.0
    ACT_FRAC = float(os.environ.get("ACTFRAC", "0.0"))
    nj = nslot // 2
    st = {}

    def load(j):
        iq_ = iqr.nxt()
        iq_v = iq_.ap.rearrange("p (g t) -> p g t", g=3)
        k.dma(iq_[0:96, :], s_IQT[:, j].rearrange("p g t -> p (g t)"), [], [iq_])
        iw_ = iwr.nxt()
        k.dma(iw_[:, :], s_IW[j * 128:(j + 1) * 128, :], [], [iw_])
        q_ = qr.nxt()
        q_bd = q_.ap.rearrange("p (f u t) -> p f u t", f=4, u=2)
        k.dma(q_bd[0:64, :, 0, :], s_QT[0:64, j], [], [q_])
        k.dma(q_bd[64:128, :, 1, :], s_QT[64:128, j], [], [q_])
        sg = sgr.nxt()
        k.act(sg[:, :], iw_[:, :], AF.Sign, [iw_], [sg])
        dgs = dgsr.nxt()
        for h in range(8):
            k.ts("pool", dgs[:, h * 128:(h + 1) * 128], identf, sg[:, h:h + 1], None, MUL, None, [cm, sg], [dgs])
        st[j] = (iq_, dgs, q_)

    def indexer(j):
        iq_, dgs, q_ = st[j]
        score = scores[j % 2]
        iq_v = iq_.ap.rearrange("p (g t) -> p g t", g=3)
        nkeys = (2 * j + 2) * 128
        for kb in range(0, nkeys, 512):
            w = min(512, nkeys - kb)
            rs = []
            for h in range(8):
                gi, pb = h // 3, 32 * (h % 3)
                bank = PB[h % 3]
                k.mm(bank[:, 0:w], iq_v[pb:pb + 32, gi, :], ikt[pb:pb + 32, kb:kb + w], True, True, [iq_, ikt], [bank])
                r = relu.nxt()
                k.act(r[:, 0:w], bank[:, 0:w], AF.Relu, [bank], [r])
                rs.append(r)
                if h >= 1:
                    hp = h - 1
                    k.mm(PB[3][:, 0:w], dgs[:, hp * 128:(hp + 1) * 128], rs[hp][:, 0:w], hp == 0, False, [dgs, rs[hp]], [PB[3]])
                if h % 2 == 1:
                    yield
            k.mm(PB[3][:, 0:w], dgs[:, 7 * 128:8 * 128], rs[7][:, 0:w], False, True, [dgs, rs[7]], [PB[3]])
            eng = "act" if (kb // 512) % 2 == 0 else "dve"
            k.cp(eng, score[:, kb:kb + w], PB[3][:, 0:w], [PB[3]], [score])
            yield
        k.tt("dve", score[:, 0:128], score[:, 0:128], dmy[:, :], ADD, [score, dmy], [score])
        k.tt("dve", score[:, nkeys - 128:nkeys], score[:, nkeys - 128:nkeys], blockmask, ADD, [score, cm], [score])

    def idx_steps(j):
        nkeys = (2 * j + 2) * 128
        return ((nkeys + 511) // 512) * 5

    def select(j, gens, exhaust=None):
        score = scores[j % 2]
        nk = 2 * j + 2
        nkeys = nk * 128
        na = int(nkeys * ACT_FRAC) // 128 * 128
        nd = nkeys - na
        k.memset("dve", mid[:, :], 0.0, [mid])
        w_ = W0
        for it in range(NITER):
            k.ts("dve", junk[:, 0:nd], score[:, 0:nd], mid[:, 0:1], None, ALU.is_ge, ADD, [score, mid], [junk, cnt],
                 accum=cnt[:, 0:1])
            if na > 0:
                k.act(negsel[:, nd:nkeys], score[:, nd:nkeys], AF.Sign, [score, mid], [negsel, sgn], scale=-1.0,
                      bias=mid[:, 0:1], accum=sgn[:, 0:1])
                k.stt(tq[:, 0:1], sgn[:, 0:1], -0.5, cnt[:, 0:1], MUL, ADD, [sgn, cnt], [tq])
                k.ts("dve", tq2[:, 0:1], tq[:, 0:1], 255.5 - na / 2.0, w_ / 2, ALU.is_ge, MUL, [tq], [tq2])
            else:
                k.ts("dve", tq2[:, 0:1], cnt[:, 0:1], 255.5, w_ / 2, ALU.is_ge, MUL, [cnt], [tq2])
            k.stt(mid[:, 0:1], tq2[:, 0:1], -w_ / 4, mid[:, 0:1], ADD, ADD, [tq2, mid], [mid])
            w_ /= 2
            for (gen, steps) in gens:
                for _ in range((steps * (it + 1)) // NITER - (steps * it) // NITER):
                    next(gen, None)
        for (gen, steps) in (gens if exhaust is None else gens[:exhaust]):
            for _ in gen:
                pass
        k.ts("dve", thr[:, 0:1], mid[:, 0:1], -w_ / 2, None, ADD, None, [mid], [thr])
        k.ts("dve", negsel[:, 0:nkeys], score[:, 0:nkeys], thr[:, 0:1], NEG, ALU.is_lt, MUL, [score, thr], [negsel])
        for g0 in range(0, nk, 4):
            n = min(4, nk - g0)
            bank = PB[3]
            bank_bf = bank.ap[:, 0:256].bitcast(BF16)
            for u in range(n):
                kt = g0 + u
                k.tr(bank_bf[:, u * 128:(u + 1) * 128], negsel[:, kt * 128:(kt + 1) * 128], identb[:, :], [negsel, identb], [bank])
            eng = "act" if cpi[0] % 2 == 0 else "dve"
            cpi[0] += 1
            k.cp(eng, nmT[:, g0 * 128:(g0 + n) * 128], bank_bf[:, 0:n * 128], [bank], [nmT])

    def attend(j):
        iq_, dgs, q_ = st[j]
        q_bd = q_.ap.rearrange("p (f c) -> p f c", f=4)
        nk = 2 * j + 2
        bO = [PB[4], PB[5]]
        for hg in range(2):
            k.mm(bO[hg][:, 0:260], zl[:, 0:128], zr[:, 0:260], True, False, [zl, zr], [bO[hg]])
        tiles = {}

        def fetch(kt):
            kt_ = ktr.nxt()
            k.dma(kt_[:, :], s_KT[:, kt].rearrange("p f t -> p (f t)"), [], [kt_])
            vt_ = vtr.nxt()
            k.dma(vt_[:, :], s_V[kt * 128:(kt + 1) * 128, :], [], [vt_])
            tiles[kt] = (kt_, vt_)

        def qk(kt, hg):
            kt_, vt_ = tiles[kt]
            kt_v = kt_.ap.rearrange("p (f t) -> p f t", f=4)
            nm4 = nmT.ap[:, kt * 128:(kt + 1) * 128].unsqueeze(1).to_broadcast([128, 4, 128])
            bS = PB[6 + hg]
            k.mm(bS.ap.rearrange("p (h t) -> p h t", h=4), identb[:, :], nm4, True, False, [identb, nmT], [bS])
            for u in range(2):
                ft = hg * 2 + u
                k.mm(bS[:, u * 256:(u + 1) * 256], kt_v[:, ft, :], q_bd[:, ft, :], False, u == 1, [kt_, q_], [bS])
            p_ = pTr.nxt()
            k.act(p_[:, :], bS[:, :], AF.Exp, [bS], [p_])
            return p_

        def pv(kt, hg, p_):
            kt_, vt_ = tiles[kt]
            for hh in range(4):
                h = hg * 4 + hh
                k.mm(bO[hg][:, hh * 65:(hh + 1) * 65], p_[:, hh * 128:(hh + 1) * 128], vt_[:, h * 65:(h + 1) * 65],
                     False, (kt == nk - 1 and hh == 3), [p_, vt_], [bO[hg]])

        units = [(kt, hg) for kt in range(nk) for hg in range(2)]
        fetch(0)
        pend = None
        for ui, (kt, hg) in enumerate(units):
            if hg == 0 and kt + 1 < nk:
                fetch(kt + 1)
            p_ = qk(kt, hg)
            if pend is not None:
                pv(*pend)
            pend = (kt, hg, p_)
            if hg == 1:
                yield
        pv(*pend)
        ya_ = yar.nxt()
        for hg in range(2):
            ov = bO[hg].ap[:, 0:260].rearrange("p (h d) -> p h d", h=4)
            k.recip(rd[:, hg * 4:(hg + 1) * 4], ov[:, :, 64], [bO[hg]], [rd])
            for hh in range(4):
                h = hg * 4 + hh
                k.ts("dve", ya_[:, h * 64:(h + 1) * 64], ov[:, hh, 0:64], rd[:, h:h + 1], None, MUL, None, [bO[hg], rd], [ya_])
        bank = PB[3]
        bank_bf = bank.ap[:, 0:256].bitcast(BF16)
        for f in range(4):
            k.tr(bank_bf[:, f * 128:(f + 1) * 128], ya_[:, f * 128:(f + 1) * 128], identb[:, :], [ya_, identb], [bank])
        yT = yTr.nxt()
        k.cp("act", yT[:, :], bank_bf[:, 0:512], [bank], [yT])
        k.dma(s_YT[:, j, 0:4, :].rearrange("p f t -> p (f t)"), yT[:, :], [yT], [])

    wgen = None
    if MOE_SORTED and os.environ.get("WCONV", "p3") == "p3":
        print("phase3 arena used before wconv:", A.off)
        wgen = wconv_gen(k, A, g, engs=("pool",))
        g["wconv_done"] = True
    wsteps = 384
    load(0)
    for _ in indexer(0):
        pass
    for j in range(nj):
        gens = []
        if j + 1 < nj:
            load(j + 1)
            gens.append((indexer(j + 1), idx_steps(j + 1)))
        if j >= 1:
            gens.append((attend(j - 1), 2 * (j - 1) + 2))
        if wgen is not None:
            tot = nj * (nj + 1)
            gens.append((wgen, (wsteps * (j + 1) * (j + 2)) // tot - (wsteps * j * (j + 1)) // tot))
        select(j, gens, exhaust=len(gens) - (1 if wgen is not None else 0))
    for _ in attend(nj - 1):
        pass
    if wgen is not None:
        for _ in wgen:
            pass
    P.barrier()


def _bcast_load(k, A, d, n):
    b = A.alloc(n, F32)
    k.dma(b[:, :], d.ap.partition_broadcast(128), [], [b])
    return b


def _layernorm(k, A, tmp, sres, gbc, bbc, outb):
    MUL, ADD, SUB = ALU.mult, ALU.add, ALU.subtract
    st, mv, ve, lnv, rstd, hn = tmp
    for hf in range(2):
        k.bn_stats(st[:, hf * 6:(hf + 1) * 6], sres[:, hf * 512:(hf + 1) * 512], [sres], [st])
    k.bn_aggr(mv[:, 0:2], st[:, 0:12], [st], [mv])
    k.ts("dve", ve[:, 0:1], mv[:, 1:2], EPS, None, ADD, None, [mv], [ve])
    k.act(lnv[:, 0:1], ve[:, 0:1], AF.Ln, [ve], [lnv])
    k.act(rstd[:, 0:1], lnv[:, 0:1], AF.Exp, [lnv], [rstd], scale=-0.5)
    k.stt(ve[:, 1:2], mv[:, 0:1], -1.0, rstd[:, 0:1], MUL, MUL, [mv, rstd], [ve])
    k.act(hn[:, :], sres[:, :], AF.Identity, [sres, rstd, ve], [hn], scale=rstd[:, 0:1], bias=ve[:, 1:2])
    k.tt("dve", hn[:, :], hn[:, :], gbc[:, :], MUL, [hn, gbc], [hn])
    k.tt("dve", outb[:, :], hn[:, :], bbc[:, :], ADD, [hn, bbc], [outb])


def build_phase4(nc, P, k, A, banks, nslot, g):
    MUL, ADD, SUB = ALU.mult, ALU.add, ALU.subtract
    A.reset()
    PB = banks()
    d_cmask, d_wout, d_wr, d_brr, d_ln1g, d_ln1b, d_xo = (g[n] for n in ("d_cmask", "d_wout", "d_wr", "d_brr", "d_ln1g", "d_ln1b", "d_xo"))
    s_YT, s_H1, s_H1T, s_GATE = (g[n] for n in ("s_YT", "s_H1", "s_H1T", "s_GATE"))
    cm = A.alloc(768, F32)
    k.dma(cm[:, :], d_cmask[:, :], [], [cm])
    identf = cm[:, 0:128]
    wout = A.alloc(8 * 1024)
    wout_v = wout.ap.rearrange("p (c f) -> p c f", c=8)
    stg = A.ring(2, 2048, F32)
    for c in range(8):
        s_ = stg.nxt()
        k.dma(s_[:, 0:1024], d_wout[c * 128:(c + 1) * 128, :], [], [s_])
        k.cp("act" if c % 2 == 0 else "dve", wout_v[:, c, :], s_[:, 0:1024], [s_], [wout])
    wr = A.alloc(8 * 36, F32)
    wr_v = wr.ap.rearrange("p (c f) -> p c f", c=8)
    for c in range(8):
        k.dma(wr_v[:, c, :], d_wr[c * 128:(c + 1) * 128, :], [], [wr])
    brr = _bcast_load(k, A, d_brr, 36)
    g1 = _bcast_load(k, A, d_ln1g, 1024)
    b1 = _bcast_load(k, A, d_ln1b, 1024)
    ytr = A.ring(2, 1024)
    xor_ = A.ring(2, 1024, F32)
    sres_l = [A.alloc(1024, F32) for _ in range(2)]
    tmp_l = [(A.alloc(12, F32), A.alloc(2, F32), A.alloc(2, F32), A.alloc(2, F32), A.alloc(2, F32), A.alloc(1024, F32))
             for _ in range(2)]
    h1r = A.ring(2, 1024, F32)
    h1Tf_l = [A.alloc(1024, F32) for _ in range(2)]
    h1Tb = A.ring(2, 1024)
    lg_l = [A.alloc(36, F32) for _ in range(2)]
    sm_l = [[A.alloc(4, F32) for _ in range(12)] for _ in range(2)]
    em_l = [A.alloc(32, F32) for _ in range(2)]
    em2_l = [A.alloc(32, F32) for _ in range(2)]
    oh1_l = [A.alloc(32, F32) for _ in range(2)]
    oh2_l = [A.alloc(32, F32) for _ in range(2)]
    gater = A.ring(2, 32, F32)
    I32 = mybir.dt.int32
    if MOE_SORTED:
        d_rc = g["d_rc"]
        s_H1B, s_TAB, s_WIDX = g["s_H1B"], g["s_TAB"], g["s_WIDX"]
        rc = A.alloc(256 + 2048 + 96 + 1 + 32, F32)
        k.dma(rc[:, :], d_rc[:, :], [], [rc])
        LTb = A.alloc(128)
        onesb = A.alloc(128)
        k.cp("dve", LTb[:, :], rc[:, 0:128], [rc], [LTb])
        k.cp("dve", onesb[:, :], rc[:, 128:256], [rc], [onesb])
        thr_ap, iot, pcol, o32 = rc[:, 256:2304], rc[:, 2304:2400], rc[:, 2400:2401], rc[:, 2401:2433]
        Wall = A.alloc(1024, F32)
        Call = A.alloc(1024, F32)
        oh1all = A.alloc(1024, F32)
        oh2all = A.alloc(1024, F32)
        g1all = A.alloc(32, F32)
        g2all = A.alloc(32, F32)
        Crun = A.alloc(32, F32)
        for b_ in (Wall, Call, oh1all, oh2all, g1all, g2all, Crun):
            k.memset("pool", b_[:, :], 0.0, [b_])
        ohs = A.alloc(32)
        h1b = A.ring(2, 1024)
    for j in range(nslot // 2):
        o0 = j * 128
        sres, tmp, h1Tf, lg, sm = sres_l[j % 2], tmp_l[j % 2], h1Tf_l[j % 2], lg_l[j % 2], sm_l[j % 2]
        em, em2, oh1, oh2 = em_l[j % 2], em2_l[j % 2], oh1_l[j % 2], oh2_l[j % 2]
        yt = ytr.nxt()
        yt_v = yt.ap.rearrange("p (f t) -> p f t", f=8)
        k.dma(yt[:, :], s_YT[:, j].rearrange("p f t -> p (f t)"), [], [yt])
        xo = xor_.nxt()
        k.dma(xo[:, :], d_xo[o0:o0 + 128, :], [], [xo])
        for hf in range(2):
            for f in range(8):
                k.mm(PB[hf][:, :], yt_v[:, f, :], wout_v[:, f, hf * 512:(hf + 1) * 512], f == 0, f == 7, [yt, wout], [PB[hf]])
        for hf in range(2):
            k.stt(sres[:, hf * 512:(hf + 1) * 512], xo[:, hf * 512:(hf + 1) * 512], ALPHA, PB[hf][:, :], MUL, ADD,
                  [xo, PB[hf]], [sres])
        h1 = h1r.nxt()
        _layernorm(k, A, tmp, sres, g1, b1, h1)
        k.dma(s_H1[o0:o0 + 128, :], h1[:, :], [h1], [])
        for c in range(8):
            k.tr(PB[2 + c // 4][:, (c % 4) * 128:(c % 4 + 1) * 128], h1[:, c * 128:(c + 1) * 128], identf, [h1, cm], [PB[2 + c // 4]])
        k.cp("act", h1Tf[:, 0:512], PB[2][:, :], [PB[2]], [h1Tf])
        k.cp("dve", h1Tf[:, 512:1024], PB[3][:, :], [PB[3]], [h1Tf])
        if not MOE_SORTED:
            hb = h1Tb.nxt()
            k.cp("pool", hb[:, :], h1Tf[:, :], [h1Tf], [hb])
            for c in range(8):
                k.dma(s_H1T[:, c, o0:o0 + 128], hb[:, c * 128:(c + 1) * 128], [hb], [])
        else:
            hb16 = h1b.nxt()
            k.cp("pool", hb16[:, :], h1[:, :], [h1], [hb16])
            k.dma(s_H1B[o0:o0 + 128, :], hb16[:, :], [hb16], [])
        h1Tf_v = h1Tf.ap.rearrange("p (c t) -> p c t", c=8)
        for c in range(8):
            k.mm(PB[4][:, 0:36], h1Tf_v[:, c, :], wr_v[:, c, :], c == 0, c == 7, [h1Tf, wr], [PB[4]])
        k.tt("dve", lg[:, :], PB[4][:, 0:36], brr[:, :], ADD, [PB[4], brr], [lg])
        gm, ngm, gsum, pg, m1, m2, dd, ee, p1, g1v, g2v, pen = sm
        k.reduce(gm[:, 0:1], lg[:, 0:4], ALU.max, [lg], [gm])
        ohg = oh2
        k.ts("dve", oh2[:, 0:4], lg[:, 0:4], gm[:, 0:1], None, ALU.is_ge, None, [lg, gm], [oh2])
        k.ts("dve", ngm[:, 0:1], gm[:, 0:1], -1.0, None, MUL, None, [gm], [ngm])
        k.act(em2[:, 0:4], lg[:, 0:4], AF.Exp, [lg, ngm], [em2, gsum], bias=ngm[:, 0:1], accum=gsum[:, 0:1])
        k.recip(pg[:, 0:1], gsum[:, 0:1], [gsum], [pg])
        k.ts("dve", pen[:, 0:4], oh2[:, 0:4], 1.0, 1e9, SUB, MUL, [oh2], [pen])
        for gi in range(4):
            k.ts("dve", em[:, gi * 8:(gi + 1) * 8], lg[:, 4 + gi * 8:4 + (gi + 1) * 8], pen[:, gi:gi + 1], None, ADD, None,
                 [lg, pen], [em])
        k.reduce(m1[:, 0:1], em[:, :], ALU.max, [em], [m1])
        k.ts("dve", oh1[:, :], em[:, :], m1[:, 0:1], None, ALU.is_ge, None, [em, m1], [oh1])
        k.stt(em2[:, :], oh1[:, :], -1e9, em[:, :], MUL, ADD, [oh1, em], [em2])
        k.reduce(m2[:, 0:1], em2[:, :], ALU.max, [em2], [m2])
        k.ts("dve", oh2[:, :], em2[:, :], m2[:, 0:1], None, ALU.is_ge, None, [em2, m2], [oh2])
        k.tt("dve", dd[:, 0:1], m2[:, 0:1], m1[:, 0:1], SUB, [m1, m2], [dd])
        k.act(ee[:, 0:1], dd[:, 0:1], AF.Exp, [dd], [ee])
        k.ts("dve", ee[:, 0:1], ee[:, 0:1], 1.0, None, ADD, None, [ee], [ee])
        k.recip(p1[:, 0:1], ee[:, 0:1], [ee], [p1])
        k.tt("dve", g1v[:, 0:1], p1[:, 0:1], pg[:, 0:1], MUL, [p1, pg], [g1v])
        k.tt("dve", g2v[:, 0:1], pg[:, 0:1], g1v[:, 0:1], SUB, [pg, g1v], [g2v])
        gt = gater.nxt()
        k.ts("dve", gt[:, :], oh1[:, :], g1v[:, 0:1], None, MUL, None, [oh1, g1v], [gt])
        k.stt(gt[:, :], oh2[:, :], g2v[:, 0:1], gt[:, :], MUL, ADD, [oh2, g2v, gt], [gt])
        if not MOE_SORTED:
            k.dma(s_GATE[o0:o0 + 128, :], gt[:, :], [gt], [])
        else:
            k.tt("dve", ohs[:, :], oh1[:, :], oh2[:, :], ADD, [oh1, oh2], [ohs])
            k.mm(PB[5][:, 0:32], LTb[:, :], ohs[:, :], True, True, [LTb, ohs], [PB[5]])
            k.cp("dve", Wall[:, j * 32:(j + 1) * 32], PB[5][:, 0:32], [PB[5]], [Wall])
            k.cp("pool", Call[:, j * 32:(j + 1) * 32], Crun[:, :], [Crun], [Call])
            k.mm(PB[5][:, 32:64], onesb[:, :], ohs[:, :], True, True, [onesb, ohs], [PB[5]])
            k.tt("dve", Crun[:, :], Crun[:, :], PB[5][:, 32:64], ADD, [Crun, PB[5]], [Crun])
            k.cp("pool", oh1all[:, j * 32:(j + 1) * 32], oh1[:, :], [oh1], [oh1all])
            k.cp("pool", oh2all[:, j * 32:(j + 1) * 32], oh2[:, :], [oh2], [oh2all])
            k.cp("pool", g1all[:, j:j + 1], g1v[:, 0:1], [g1v], [g1all])
            k.cp("pool", g2all[:, j:j + 1], g2v[:, 0:1], [g2v], [g2all])
    if MOE_SORTED:
        ntile = nslot // 2
        v3 = lambda b_: b_.ap.rearrange("p (a b) -> p a b", a=32)
        cmpt = A.alloc(2048, F32)
        k.tt("dve", v3(cmpt), thr_ap.rearrange("p (a b) -> p a b", a=32), Crun.ap.unsqueeze(2).to_broadcast([128, 32, 64]),
             ALU.is_lt, [rc, Crun], [cmpt])
        ceil_ = A.alloc(32, F32)
        k.reduce(ceil_[:, :], v3(cmpt), ALU.add, [cmpt], [ceil_])
        padded = A.alloc(32, F32)
        k.ts("dve", padded[:, :], ceil_[:, :], 128.0, None, MUL, None, [ceil_], [padded])
        incl = A.alloc(32, F32)
        k.scan(incl[:, :], o32, padded[:, :], 0.0, MUL, ADD, [rc, padded], [incl])
        offs = A.alloc(32, F32)
        k.tt("dve", offs[:, :], incl[:, :], padded[:, :], SUB, [incl, padded], [offs])
        endt = A.alloc(32, F32)
        k.ts("dve", endt[:, :], incl[:, :], 1.0 / 128.0, None, MUL, None, [incl], [endt])
        texp = A.alloc(NTS, F32)
        k.memset("dve", texp[:, :], 0.0, [texp])
        for e in range(32):
            k.stt(texp[:, :], iot, endt[:, e:e + 1], texp[:, :], ALU.is_ge, ADD, [rc, endt, texp], [texp])
        k.ts("dve", texp[:, :], texp[:, :], 31.0, None, ALU.min, None, [texp], [texp])
        widxf = A.alloc(NTS, F32)
        k.ts("dve", widxf[:, :], texp[:, :], 128.0, pcol, MUL, ADD, [texp, rc], [widxf])
        same = A.alloc(NTS, F32)
        k.memset("dve", same[:, :], 0.0, [same])
        k.tt("dve", same[:, 2:NTS], texp[:, 2:NTS], texp[:, 0:NTS - 2], ALU.is_equal, [texp], [same])
        k.stt(widxf[:, :], same[:, :], 4096.0, widxf[:, :], MUL, ADD, [same, widxf], [widxf])
        widx = A.alloc(NTS, I32)
        k.cp("dve", widx[:, :], widxf[:, :], [widxf], [widx])
        k.dma(s_WIDX.ap, widx[:, :], [widx], [])
        roff = A.alloc(1024, F32)
        k.tt("dve", roff[:, :], Wall[:, :], Call[:, :], ADD, [Wall, Call], [roff])
        k.tt("dve", v3(roff), v3(roff), offs.ap.unsqueeze(1).to_broadcast([128, 32, 32]), ADD, [roff, offs], [roff])
        tmpm = A.alloc(1024, F32)
        posf = A.alloc(64, F32)
        k.tt("dve", tmpm[:, :], roff[:, :], oh1all[:, :], MUL, [roff, oh1all], [tmpm])
        k.reduce(posf[:, 0:32], v3(tmpm), ALU.add, [tmpm], [posf])
        k.tt("dve", tmpm[:, :], roff[:, :], oh2all[:, :], MUL, [roff, oh2all], [tmpm])
        k.reduce(posf[:, 32:64], v3(tmpm), ALU.add, [tmpm], [posf])
        posi = A.alloc(64, I32)
        k.cp("dve", posi[:, :], posf[:, :], [posf], [posi])
        ent = A.alloc(64 * 16, I32)
        k.memset("pool", ent[:, :], 0, [ent])
        tokf = A.alloc(32, F32)
        k.ts("dve", tokf[:, :], iot[:, 0:32], 128.0, pcol, MUL, ADD, [rc], [tokf])
        tokf2 = A.alloc(32, F32)
        k.ts("dve", tokf2[:, :], tokf[:, :], float(TO), None, ADD, None, [tokf], [tokf2])
        ent_v = ent.ap.rearrange("p (k c) -> p k c", c=16)
        entf_v = ent.ap.bitcast(F32).rearrange("p (k c) -> p k c", c=16)
        k.cp("dve", ent_v[:, 0:32, 0], tokf[:, :], [tokf], [ent])
        k.cp("dve", ent_v[:, 32:64, 0], tokf[:, :], [tokf], [ent])
        td = A.alloc(32, F32)
        for (c0_, src_, add_) in ((0, tokf, 0.0), (32, tokf2, 0.0)):
            k.ts("dve", td[:, :], src_[:, :], 2.0, None, MUL, None, [src_], [td])
            k.cp("dve", ent_v[:, c0_:c0_ + 32, 1], td[:, :], [td], [ent])
            k.ts("dve", td[:, :], td[:, :], 1.0, None, ADD, None, [td], [td])
            k.cp("dve", ent_v[:, c0_:c0_ + 32, 3], td[:, :], [td], [ent])
        k.cp("dve", entf_v[:, 0:32, 2], g1all[:, :], [g1all], [ent])
        k.cp("dve", entf_v[:, 32:64, 2], g2all[:, :], [g2all], [ent])
        tabi = A.alloc(NTS * 16, I32)
        k.memset("pool", tabi[:, :], 0, [tabi])
        k.memset("pool", tabi.ap.rearrange("p (t c) -> p t c", c=16)[:, :, 1], 1000000, [tabi])
        k.memset("pool", tabi.ap.rearrange("p (t c) -> p t c", c=16)[:, :, 3], 1000000, [tabi])
        k.dma(s_TAB.ap.rearrange("(p t) c -> p (t c)", p=128), tabi[:, :], [tabi], [s_TAB])
        for sl_ in range(2):
            for jj in range(ntile):
                kk = sl_ * 32 + jj
                k.scatter(s_TAB.ap[:, :], ent_v[:, kk, :], posi[:, kk:kk + 1], NTS * 128 - 1, [ent, posi, s_TAB], [s_TAB])
    P.barrier()


def build_phase5(nc, P, k, A, banks, nslot, nexp, g):
    MUL, ADD = ALU.mult, ALU.add
    A.reset()
    PB = banks()
    d_wein, d_weout, d_ln2g, d_ln2b, d_out = (g[n] for n in ("d_wein", "d_weout", "d_ln2g", "d_ln2b", "d_out"))
    s_H1, s_H1T, s_GATE = (g[n] for n in ("s_H1", "s_H1T", "s_GATE"))
    g2 = _bcast_load(k, A, d_ln2g, 1024)
    b2 = _bcast_load(k, A, d_ln2b, 1024)
    ntile = nslot // 2
    TB = min(16, ntile)
    nblk = ntile // TB
    acc = A.alloc(TB * 1024, F32)
    acc_v = acc.ap.rearrange("p (t f) -> p t f", t=TB)
    hT = A.alloc(8 * TB * 128)
    hT_v = hT.ap.rearrange("p (c t) -> p c t", c=8)
    gt = A.alloc(TB * 32, F32)
    gt_v = gt.ap.rearrange("p (t e) -> p t e", t=TB)
    winr = A.ring(2, 8 * 1024)
    woutr = A.ring(2, 4 * 1024)
    stg = A.ring(3, 1024, F32)
    aTr = A.ring(2, 4 * 512)
    silr = A.ring(2, 512, F32)
    h1r = A.ring(1, 1024, F32)
    sres = A.alloc(1024, F32)
    tmp = (A.alloc(12, F32), A.alloc(2, F32), A.alloc(2, F32), A.alloc(2, F32), A.alloc(2, F32), A.alloc(1024, F32))
    outr = A.ring(1, 1024, F32)
    ybank = [0]
    for blk in range(nblk):
        t0 = blk * TB * 128
        for c in range(8):
            k.dma(hT_v[:, c, :], s_H1T[:, c, t0:t0 + TB * 128], [], [hT])
        for t in range(TB):
            k.dma(gt_v[:, t, :], s_GATE[t0 + t * 128:t0 + (t + 1) * 128, :], [], [gt])
        for e in range(nexp):
            win = winr.nxt()
            win_v = win.ap.rearrange("p (c f) -> p c f", c=8)
            for c in range(8):
                s_ = stg.nxt()
                k.dma(s_[:, :], d_wein.ap[e][c * 128:(c + 1) * 128, :], [], [s_])
                k.cp("pool", win_v[:, c, :], s_[:, :], [s_], [win])
            wo = woutr.nxt()
            wo_v = wo.ap.rearrange("p (c f) -> p c f", c=4)
            for c in range(4):
                s_ = stg.nxt()
                k.dma(s_[:, :], d_weout.ap[e][c * 128:(c + 1) * 128, :], [], [s_])
                k.cp("pool", wo_v[:, c, :], s_[:, :], [s_], [wo])
            for sb in range(0, TB, 4):
                nt = min(4, TB - sb)
                ncol = nt * 128
                aT = aTr.nxt()
                aT_v = aT.ap.rearrange("p (f t) -> p f t", f=4)
                for ft in range(4):
                    bg_, bu_ = PB[(ft % 2) * 2], PB[(ft % 2) * 2 + 1]
                    for c in range(8):
                        k.mm(bg_[:, 0:ncol], win_v[:, c, ft * 128:(ft + 1) * 128], hT_v[:, c, sb * 128:sb * 128 + ncol],
                             c == 0, c == 7, [win, hT], [bg_])
                    for c in range(8):
                        k.mm(bu_[:, 0:ncol], win_v[:, c, 512 + ft * 128:512 + (ft + 1) * 128], hT_v[:, c, sb * 128:sb * 128 + ncol],
                             c == 0, c == 7, [win, hT], [bu_])
                    sl_ = silr.nxt()
                    k.act(sl_[:, 0:ncol], bg_[:, 0:ncol], AF.Silu, [bg_], [sl_])
                    k.tt("dve", aT_v[:, ft, 0:ncol], sl_[:, 0:ncol], bu_[:, 0:ncol], MUL, [sl_, bu_], [aT])
                for u in range(nt):
                    t = sb + u
                    yb0, yb1 = PB[4 + (ybank[0] % 2) * 2], PB[5 + (ybank[0] % 2) * 2]
                    ybank[0] += 1
                    for hf, yb_ in enumerate((yb0, yb1)):
                        for fc in range(4):
                            k.mm(yb_[:, :], aT_v[:, fc, u * 128:(u + 1) * 128], wo_v[:, fc, hf * 512:(hf + 1) * 512],
                                 fc == 0, fc == 3, [aT, wo], [yb_])
                    for hf, yb_ in enumerate((yb0, yb1)):
                        dst = acc_v[:, t, hf * 512:(hf + 1) * 512]
                        if e == 0:
                            k.ts("dve", dst, yb_[:, :], gt_v[:, t, e:e + 1], None, MUL, None, [yb_, gt], [acc])
                        else:
                            k.stt(dst, yb_[:, :], gt_v[:, t, e:e + 1], dst, MUL, ADD, [yb_, gt, acc], [acc])
        for t in range(TB):
            r0 = t0 + t * 128
            h1 = h1r.nxt()
            k.dma(h1[:, :], s_H1[r0:r0 + 128, :], [], [h1])
            k.stt(sres[:, :], h1[:, :], ALPHA, acc_v[:, t, :], MUL, ADD, [h1, acc], [sres])
            ob = outr.nxt()
            _layernorm(k, A, tmp, sres, g2, b2, ob)
            k.dma(d_out[r0:r0 + 128, :], ob[:, :], [ob], [], final=True)


def wconv_gen(k, A, g, engs=("act", "dve", "pool")):
    d_wein, d_weout, s_WBI, s_WBO = g["d_wein"], g["d_weout"], g["s_WBI"], g["s_WBO"]
    stg = A.ring(3, 1024, F32)
    stb = A.ring(3, 1024)
    i = 0
    for e in range(32):
        for (dsrc, ddst, nch) in ((d_wein, s_WBI, 8), (d_weout, s_WBO, 4)):
            for c in range(nch):
                s_ = stg.nxt()
                b_ = stb.nxt()
                k.dma(s_[:, :], dsrc.ap[e][c * 128:(c + 1) * 128, :], [], [s_])
                k.cp(engs[i % len(engs)], b_[:, :], s_[:, :], [s_], [b_])
                i += 1
                k.dma(ddst[e * 128:(e + 1) * 128, c * 1024:(c + 1) * 1024], b_[:, :], [b_], [])
                yield


def build_phase5s(nc, P, k, A, banks, nslot, g):
    MUL, ADD = ALU.mult, ALU.add
    I32 = mybir.dt.int32
    A.reset()
    PB = banks()
    d_cmask, d_ln2g, d_ln2b, d_out = (g[n] for n in ("d_cmask", "d_ln2g", "d_ln2b", "d_out"))
    s_H1, s_H1B, s_WBI, s_WBO, s_TAB, s_WIDX, s_Y2 = (g[n] for n in ("s_H1", "s_H1B", "s_WBI", "s_WBO", "s_TAB", "s_WIDX", "s_Y2"))
    if not g.get("wconv_done"):
        for _ in wconv_gen(k, A, g):
            pass
        P.barrier()
        A.reset()
    cm = A.alloc(768, F32)
    k.dma(cm[:, :], d_cmask[:, :], [], [cm])
    identb = A.alloc(128)
    k.cp("dve", identb[:, :], cm[:, 0:128], [cm], [identb])
    g2 = _bcast_load(k, A, d_ln2g, 1024)
    b2 = _bcast_load(k, A, d_ln2b, 1024)
    widx = A.alloc(NTS, I32)
    k.dma(widx[:, :], s_WIDX.ap, [], [widx])
    tabr = Ring([Buf(t_[:, :]) for t_ in g["tab_t"]])
    xsr = A.ring(2, 1024)
    winr = A.ring(2, 8192)
    wor = A.ring(2, 4096)
    xTr = A.ring(2, 1024)
    silr = A.ring(2, 512, F32)
    ar = A.ring(2, 512)
    aTr = A.ring(2, 512)
    ysr = A.ring(2, 1024, F32)
    ntile = nslot // 2
    nts = min(NTS, 2 * ntile + 32)
    staged = {}

    def fetch(t):
        tab = tabr.nxt()
        k.dma(tab[:, :], s_TAB.ap[t * 128:(t + 1) * 128, :], [s_TAB], [tab])
        xs = xsr.nxt()
        k.gather(xs[:, :], s_H1B.ap[0:(nslot // 2) * 128, :], tab[:, 0:1], [tab], [xs])
        win = winr.nxt()
        k.gather(win[:, :], s_WBI.ap[:, :], widx[:, t:t + 1], [widx], [win], bound=32 * 128 - 1)
        wo = wor.nxt()
        k.gather(wo[:, :], s_WBO.ap[:, :], widx[:, t:t + 1], [widx], [wo], bound=32 * 128 - 1)
        staged[t] = (tab, xs, win, wo)

    fetch(0)
    for t in range(nts):
        if t + 1 < nts:
            fetch(t + 1)
        tab, xs, win, wo = staged.pop(t)
        win_v = win.ap.rearrange("p (c f) -> p c f", c=8)
        wo_v = wo.ap.rearrange("p (c f) -> p c f", c=4)
        bT = PB[0]
        bT_bf = bT.ap.bitcast(BF16)
        for c in range(8):
            k.tr(bT_bf[:, c * 128:(c + 1) * 128], xs[:, c * 128:(c + 1) * 128], identb[:, :], [xs, identb], [bT])
        xT = xTr.nxt()
        k.cp("act", xT[:, 0:512], bT_bf[:, 0:512], [bT], [xT])
        k.cp("dve", xT[:, 512:1024], bT_bf[:, 512:1024], [bT], [xT])
        xT_v = xT.ap.rearrange("p (c t) -> p c t", c=8)
        bg_, bu_ = PB[1 + 2 * (t % 2)], PB[2 + 2 * (t % 2)]
        for c in range(8):
            k.mm(bg_[:, :], xT_v[:, c, :], win_v[:, c, 0:512], c == 0, c == 7, [xT, win], [bg_])
        for c in range(8):
            k.mm(bu_[:, :], xT_v[:, c, :], win_v[:, c, 512:1024], c == 0, c == 7, [xT, win], [bu_])
        sl_ = silr.nxt()
        k.act(sl_[:, :], bg_[:, :], AF.Silu, [bg_], [sl_])
        a_ = ar.nxt()
        gate = tab.ap[:, 2:3].bitcast(F32)
        k.stt(a_[:, :], sl_[:, :], gate, bu_[:, :], MUL, MUL, [sl_, tab, bu_], [a_])
        bA = PB[5]
        bA_bf = bA.ap[:, 0:256].bitcast(BF16)
        for fc in range(4):
            k.tr(bA_bf[:, fc * 128:(fc + 1) * 128], a_[:, fc * 128:(fc + 1) * 128], identb[:, :], [a_, identb], [bA])
        aT = aTr.nxt()
        k.cp("act", aT[:, :], bA_bf[:, 0:512], [bA], [aT])
        aT_v = aT.ap.rearrange("p (f t) -> p f t", f=4)
        ys = ysr.nxt()
        for hf in range(2):
            by = PB[6 + hf]
            for fc in range(4):
                k.mm(by[:, :], aT_v[:, fc, :], wo_v[:, fc, hf * 512:(hf + 1) * 512], fc == 0, fc == 3, [aT, wo], [by])
            k.cp("act" if hf == 0 else "dve", ys[:, hf * 512:(hf + 1) * 512], by[:, :], [by], [ys])
        y2v = s_Y2.ap.rearrange("r (h f) -> (r h) f", h=2)
        k.scatter(y2v, ys[:, 0:512], tab[:, 1:2], 4 * TO - 1, [ys, tab], [])
        k.scatter(y2v, ys[:, 512:1024], tab[:, 3:4], 4 * TO - 1, [ys, tab], [])
    P.barrier()
    h1r = A.ring(2, 1024, F32)
    y2r = A.ring(2, 2048, F32)
    sres_l = [A.alloc(1024, F32) for _ in range(2)]
    tmp_l = [(A.alloc(12, F32), A.alloc(2, F32), A.alloc(2, F32), A.alloc(2, F32), A.alloc(2, F32), A.alloc(1024, F32))
             for _ in range(2)]
    outr = A.ring(2, 1024, F32)
    for t in range(ntile):
        r0 = t * 128
        sres, tmp = sres_l[t % 2], tmp_l[t % 2]
        h1 = h1r.nxt()
        k.dma(h1[:, :], s_H1[r0:r0 + 128, :], [], [h1])
        y2 = y2r.nxt()
        k.dma(y2[:, 0:1024], s_Y2[r0:r0 + 128, :], [], [y2])
        k.dma(y2[:, 1024:2048], s_Y2[TO + r0:TO + r0 + 128, :], [], [y2])
        k.stt(sres[:, :], h1[:, :], ALPHA, y2[:, 0:1024], MUL, ADD, [h1, y2], [sres])
        k.tt("pool", sres[:, :], sres[:, :], y2[:, 1024:2048], ADD, [sres, y2], [sres])
        ob = outr.nxt()
        _layernorm(k, A, tmp, sres, g2, b2, ob)
        k.dma(d_out[r0:r0 + 128, :], ob[:, :], [ob], [], final=True)


_OFF = np.cumsum([0, 512, 512, 512, 256, 32, 8, 256, 256, 512, 512, 16])
O_AQ, O_AK, O_AV, O_IQ, O_IK, O_IW, O_BQ, O_BK, O_BV, O_BR, O_BG = [int(v) for v in _OFF[:11]]


def _consts():
    j = np.arange(128)[:, None]
    i = np.arange(128)[None, :]
    same = (j // 64) == (i // 64)
    ident = np.eye(128, dtype=np.float32)
    triinc = np.where(same & (j <= i), -1.0 / 16.0, 0.0).astype(np.float32)
    trirev = np.where(same & (j > i), -1.0 / 16.0, 0.0).astype(np.float32)
    atmask = np.where(same & (j <= i), 1.0, 0.0).astype(np.float32)
    blockmask = np.where((j < 64) & (i >= 64), -1e30, 0.0).astype(np.float32)
    sel64 = np.zeros((128, 128), np.float32)
    sel64[64, :64] = 1.0
    return np.concatenate([ident, triinc, trirev, atmask, blockmask, sel64], axis=1)


def _rconsts():
    j = np.arange(128)[:, None]
    i = np.arange(128)[None, :]
    lt = (j < i).astype(np.float32)
    ones = np.ones((128, 128), np.float32)
    thr = np.tile((np.arange(64, dtype=np.float32) * 128.0)[None, :], (32, 1)).reshape(1, 2048)
    thr = np.tile(thr, (128, 1))
    iot = np.tile(np.arange(96, dtype=np.float32)[None, :], (128, 1))
    pcol = np.arange(128, dtype=np.float32)[:, None]
    o32 = np.ones((128, 32), np.float32)
    return np.ascontiguousarray(np.concatenate([lt, ones, thr, iot, pcol, o32], axis=1))


def _weight_layouts(w_in):
    w = np.asarray(w_in[0], np.float32)
    z = np.zeros((1024, 128), np.float32)

    def tile(cols):
        t = z.copy()
        t[:, :len(cols)] = w[:, cols]
        return t

    def rot(base, nh, dh):
        cols = []
        for h in range(nh):
            b = base + h * dh
            cols += list(range(b + dh // 2, b + dh)) + list(range(b, b + dh // 2))
        return cols

    fk = []
    for i in range(4):
        fk.append(tile(list(range(O_AK + i * 128, O_AK + (i + 1) * 128))))
    for i in range(4):
        fk.append(tile(rot(O_AK + i * 128, 2, 64)))
    ik = list(range(O_IK, O_IK + 32))
    fk.append(tile(ik * 3))
    fk.append(tile(rot(O_IK, 1, 32) * 3))
    for ft in range(2):
        fk.append(tile(list(range(O_BK + ft * 128, O_BK + (ft + 1) * 128))))
    fk.append(tile(list(range(O_BG, O_BG + 16))))
    fq = []
    for i in range(4):
        fq.append(tile(list(range(O_AQ + i * 128, O_AQ + (i + 1) * 128))))
    for i in range(4):
        fq.append(tile(rot(O_AQ + i * 128, 2, 64)))
    groups = [(0, 3), (3, 3), (6, 2)]
    for (h0, n) in groups:
        fq.append(tile(list(range(O_IQ + h0 * 32, O_IQ + (h0 + n) * 32))))
    for (h0, n) in groups:
        fq.append(tile(rot(O_IQ + h0 * 32, n, 32)))
    for ft in range(2):
        fq.append(tile(list(range(O_BQ + ft * 128, O_BQ + (ft + 1) * 128))))
    for (h0, n) in groups:
        cols = []
        for h in range(h0, h0 + n):
            cols += [O_IW + h] * 32
        fq.append(tile(cols))
    wfk = np.ascontiguousarray(np.concatenate(fk, axis=1))
    wfq = np.ascontiguousarray(np.concatenate(fq, axis=1))
    wtk = np.ascontiguousarray(np.concatenate([w[:, O_AV:O_AV + 512], w[:, O_BV:O_BV + 512], w[:, O_BK:O_BK + 256]], axis=1))
    wtq = np.ascontiguousarray(np.concatenate([w[:, O_BR:O_BR + 512], w[:, O_IW:O_IW + 8]], axis=1))
    return wfk, wfq, wtk, wtq


def _rope_tables(pos):
    pos = pos.astype(np.float32)
    inv32 = (1.0 / (np.float32(10000.0) ** (np.arange(32, dtype=np.float32) / np.float32(32)))).astype(np.float32)
    inv16 = (1.0 / (np.float32(10000.0) ** (np.arange(16, dtype=np.float32) / np.float32(16)))).astype(np.float32)
    p = np.arange(128)
    angA = (pos[None, :] * inv32[(p % 64) % 32][:, None]).astype(np.float32)
    sgnA = np.where((p % 64) < 32, -1.0, 1.0).astype(np.float32)[:, None]
    angI = (pos[None, :] * inv16[(p % 32) % 16][:, None]).astype(np.float32)
    sgnI = np.where((p % 32) < 16, -1.0, 1.0).astype(np.float32)[:, None]
    return (np.cos(angA).astype(np.float32), (np.sin(angA) * sgnA).astype(np.float32),
            np.cos(angI).astype(np.float32), (np.sin(angI) * sgnI).astype(np.float32))


def prep_inputs(inputs, nslot=NSLOT):
    x = np.asarray(inputs["x"], np.float32)
    wfk, wfq, wtk, wtq = _weight_layouts(inputs["w_in"])
    wg = np.concatenate([np.asarray(inputs["w_gla_gate"][0], np.float32),
                         np.asarray(inputs["b_gla_gate"], np.float32).reshape(1, 256)], axis=0)
    wr = np.concatenate([np.asarray(inputs["w_group_router"][0], np.float32),
                         np.asarray(inputs["w_expert_router"][0], np.float32)], axis=1)
    brr = np.concatenate([np.asarray(inputs["b_group_router"], np.float32).reshape(1, 4),
                          np.asarray(inputs["b_expert_router"], np.float32).reshape(1, 32)], axis=1)
    common = {
        "wfk": wfk, "wfq": wfq, "wtk": wtk, "wtq": wtq, "wg": np.ascontiguousarray(wg),
        "gn": np.asarray(inputs["g_gla_norm"], np.float32).reshape(1, 128),
        "wout": np.ascontiguousarray(inputs["w_out"][0], dtype=np.float32),
        "ln1g": np.asarray(inputs["ln1_g"], np.float32).reshape(1, 1024),
        "ln1b": np.asarray(inputs["ln1_b"], np.float32).reshape(1, 1024),
        "wr": np.ascontiguousarray(wr), "brr": brr,
        "wein": np.ascontiguousarray(inputs["w_expert_in"][0], dtype=np.float32),
        "weout": np.ascontiguousarray(inputs["w_expert_out"][0], dtype=np.float32),
        "ln2g": np.asarray(inputs["ln2_g"], np.float32).reshape(1, 1024),
        "ln2b": np.asarray(inputs["ln2_b"], np.float32).reshape(1, 1024),
        "cmask": _consts(),
        "rconst": _rconsts(),
    }
    maps = []
    for c in range(8):
        b, half = c // 2, c % 2
        xb = x[b]
        if half == 0:
            xs = np.concatenate([np.zeros((128, 1024), np.float32), xb[:T - 128]], axis=0)
            pos = np.concatenate([np.zeros(128), np.arange(T - 128)])
        else:
            xs = xb
            pos = np.arange(T)
        ca, sa, ci, si = _rope_tables(pos)
        xo = xs.reshape(NSLOT, 128, 1024)[1::2].reshape(TO, 1024)
        m = dict(common)
        m["xT"] = np.ascontiguousarray(xs.T)
        m["xo"] = np.ascontiguousarray(xo)
        m["ca"], m["sa"], m["ci"], m["si"] = ca, sa, ci, si
        m["dummyb"] = np.full((128, 128), -1e30 if half == 0 else 0.0, np.float32)
        maps.append(m)
    return maps


def assemble(outs):
    y = np.zeros((4, T, 1024), np.float32)
    for c in range(8):
        b, half = c // 2, c % 2
        o = np.asarray(outs[c], np.float32).reshape(32, 128, 1024)
        yv = y[b].reshape(NSLOT, 128, 1024)
        if half == 0:
            yv[0::2] = o
        else:
            yv[1::2] = o
    return y


_NC_CACHE = {}


def kernel(**inputs):
    maps = prep_inputs(inputs)
    if "nc" not in _NC_CACHE:
        _NC_CACHE["nc"] = build()
    res = run_bass_kernel_spmd(_NC_CACHE["nc"], maps, core_ids=list(range(8)))
    return assemble([r["out"] for r in res.results])
```

```python
import os
import numpy as np
from contextlib import ExitStack
import concourse.bass as bass
import concourse.mybir as mybir
from concourse.bass_utils import run_bass_kernel_spmd

F32 = mybir.dt.float32
BF16 = mybir.dt.bfloat16
AF = mybir.ActivationFunctionType
ALU = mybir.AluOpType

NSLOT = 64
T = 8192
TO = 4096
NEG = -30000.0
EPS = 1e-5
ALPHA = 2.0 ** 0.25
NIT = 24


class Res:
    __slots__ = ("w", "r")

    def __init__(self):
        self.w = None
        self.r = []


class Op:
    __slots__ = ("eng", "fn", "deps", "idx", "signal", "sem", "val", "dma", "presem")

    def __init__(self, eng, fn, dma):
        self.eng = eng
        self.fn = fn
        self.dma = dma
        self.deps = []
        self.signal = False
        self.sem = None
        self.val = 0
        self.presem = None


class Prog:
    ENGS = ("pe", "act", "dve", "pool", "sp")
    CHUNK = 30000
    NDMASEM = int(os.environ.get("NDMASEM", "8"))

    def __init__(self, nc):
        self.nc = nc
        self.ops = {e: [] for e in self.ENGS}
        self.outs = []
        self.dmas = []

    def add(self, eng, fn, reads=(), writes=(), dma=False, out=False):
        op = Op(eng, fn, dma)
        deps = {}
        for r in reads:
            if r.w is not None:
                deps[id(r.w)] = r.w
        for w in writes:
            if w.w is not None:
                deps[id(w.w)] = w.w
            for rr in w.r:
                deps[id(rr)] = rr
        best = {}
        lst = []
        for d in deps.values():
            if d.dma:
                lst.append(d)
            else:
                if d.eng == "pe" and eng == "pe" and not dma:
                    continue
                b = best.get(d.eng)
                if b is None or d.idx > b.idx:
                    best[d.eng] = d
        lst.extend(best.values())
        op.deps = lst
        for d in lst:
            d.signal = True
        op.idx = len(self.ops[eng])
        self.ops[eng].append(op)
        for r in reads:
            r.r.append(op)
        for w in writes:
            w.w = op
            w.r = []
        if dma:
            self.dmas.append(op)
        if out:
            op.signal = True
            self.outs.append(op)
        return op

    def barrier(self):
        lasts = []
        for e in self.ENGS:
            for op in reversed(self.ops[e]):
                if not op.dma and op.fn is not None:
                    op.signal = True
                    lasts.append(op)
                    break
        for d in self.dmas:
            d.signal = True
        deps = lasts + self.dmas
        self.dmas = []
        for e in self.ENGS:
            op = Op(e, None, False)
            op.deps = list(deps)
            op.idx = len(self.ops[e])
            self.ops[e].append(op)

    def emit(self, es):
        nc = self.nc
        for e in self.ENGS:
            cnt = 0
            sem = None
            k = 0
            dsems = []
            dcnt = []
            di = 0
            for op in self.ops[e]:
                if not op.signal:
                    continue
                if op.dma:
                    j = di % self.NDMASEM
                    di += 1
                    if j >= len(dsems):
                        dsems.append(es.enter_context(nc.semaphore(f"d_{e}_{len(dsems)}")))
                        dcnt.append(0)
                    op.sem = dsems[j]
                    if dcnt[j] > 0:
                        op.presem = (dsems[j], 16 * dcnt[j])
                    dcnt[j] += 1
                    op.val = 16 * dcnt[j]
                else:
                    if sem is None or cnt >= self.CHUNK:
                        sem = es.enter_context(nc.semaphore(f"c_{e}_{k}"))
                        k += 1
                        cnt = 0
                    cnt += 1
                    op.sem = sem
                    op.val = cnt
        fin = Op("sp", None, False)
        fin.deps = list(self.outs)
        self.ops["sp"].append(fin)
        block = es.enter_context(nc.Block())
        prog = self

        def run(e, eng):
            waited = {}
            for op in prog.ops[e]:
                need = {}
                for d in op.deps:
                    key = id(d.sem)
                    if need.get(key, (None, 0))[1] < d.val:
                        need[key] = (d.sem, d.val)
                if op.presem is not None:
                    key = id(op.presem[0])
                    if need.get(key, (None, 0))[1] < op.presem[1]:
                        need[key] = op.presem
                for key, (s, v) in need.items():
                    if waited.get(key, 0) >= v:
                        continue
                    waited[key] = v
                    eng.wait_ge(s, v)
                if op.fn is None:
                    continue
                ins = op.fn(eng)
                if op.signal:
                    ins.then_inc(op.sem, 16 if op.dma else 1)

        @block.tensor
        def _(eng):
            run("pe", eng)

        @block.scalar
        def _(eng):
            run("act", eng)

        @block.vector
        def _(eng):
            run("dve", eng)

        @block.gpsimd
        def _(eng):
            run("pool", eng)

        @block.sync
        def _(eng):
            run("sp", eng)


class Buf:
    def __init__(self, ap, tracked=True):
        self.ap = ap
        self.res = Res()
        self.tracked = tracked

    def __getitem__(self, k):
        return self.ap[k]


def _rl(bs):
    return [b.res for b in bs if b.tracked]


class Ring:
    def __init__(self, bufs):
        self.bufs = bufs
        self.i = 0

    def nxt(self):
        b = self.bufs[self.i % len(self.bufs)]
        self.i += 1
        return b


class K:
    def __init__(self, P):
        self.P = P

    def mm(self, out, lhsT, rhs, start, stop, R, W):
        self.P.add("pe", lambda e: e.matmul(out, lhsT=lhsT, rhs=rhs, start=start, stop=stop),
                   _rl(R), _rl(W))

    def tr(self, out, in_, ident, R, W):
        self.P.add("pe", lambda e: e.transpose(out=out, in_=in_, identity=ident),
                   _rl(R), _rl(W))

    def act(self, out, in_, func, R, W, scale=1.0, bias=None, accum=None):
        kw = {}
        if bias is not None:
            kw["bias"] = bias
        if accum is not None:
            kw["accum_out"] = accum
        self.P.add("act", lambda e: e.activation(out=out, in_=in_, func=func, scale=scale, **kw),
                   _rl(R), _rl(W))

    def tt(self, eng, out, in0, in1, op, R, W):
        self.P.add(eng, lambda e: e.tensor_tensor(out=out, in0=in0, in1=in1, op=op),
                   _rl(R), _rl(W))

    def ts(self, eng, out, in0, s1, s2, op0, op1, R, W, accum=None):
        kw = {}
        if accum is not None:
            kw["accum_out"] = accum
        if op1 is None:
            self.P.add(eng, lambda e: e.tensor_scalar(out=out, in0=in0, scalar1=s1, scalar2=None, op0=op0, **kw),
                       _rl(R), _rl(W))
        else:
            self.P.add(eng, lambda e: e.tensor_scalar(out=out, in0=in0, scalar1=s1, scalar2=s2, op0=op0, op1=op1, **kw),
                       _rl(R), _rl(W))

    def stt(self, out, in0, scalar, in1, op0, op1, R, W):
        self.P.add("dve", lambda e: e.scalar_tensor_tensor(out=out, in0=in0, scalar=scalar, in1=in1, op0=op0, op1=op1),
                   _rl(R), _rl(W))

    def cp(self, eng, out, in_, R, W):
        if eng == "act":
            self.P.add("act", lambda e: e.copy(out=out, in_=in_), _rl(R), _rl(W))
        else:
            self.P.add(eng, lambda e: e.tensor_copy(out=out, in_=in_), _rl(R), _rl(W))

    def memset(self, eng, ap, val, W):
        self.P.add(eng, lambda e: e.memset(ap, val), [], _rl(W))

    def dma(self, out, in_, R, W, final=False):
        self.P.add("sp", lambda e: e.dma_start(out=out, in_=in_), _rl(R), _rl(W),
                   dma=True, out=final)

    def reduce(self, out, in_, op, R, W):
        self.P.add("dve", lambda e: e.tensor_reduce(out=out, in_=in_, axis=mybir.AxisListType.X, op=op), _rl(R), _rl(W))

    def gather(self, out, in_, idx, R, W, bound=None):
        regs = self.__dict__.setdefault("_bregs", {})

        def fn(e):
            if bound is None:
                return e.indirect_dma_start(out=out, out_offset=None, in_=in_,
                                            in_offset=bass.IndirectOffsetOnAxis(ap=idx, axis=0))
            if bound not in regs:
                regs[bound] = e.to_reg(bound)
            return e.indirect_dma_start(out=out, out_offset=None, in_=in_,
                                        in_offset=bass.IndirectOffsetOnAxis(ap=idx, axis=0),
                                        bounds_check=regs[bound], oob_is_err=False)

        self.P.add("pool", fn, _rl(R), _rl(W), dma=True)

    def scatter(self, out, in_, idx, bound, R, W):
        regs = self.__dict__.setdefault("_bregs", {})

        def fn(e):
            if bound not in regs:
                regs[bound] = e.to_reg(bound)
            return e.indirect_dma_start(out=out, out_offset=bass.IndirectOffsetOnAxis(ap=idx, axis=0),
                                        in_=in_, in_offset=None, bounds_check=regs[bound], oob_is_err=False)

        self.P.add("pool", fn, _rl(R), _rl(W), dma=True)

    def scan(self, out, d0, d1, init, op0, op1, R, W):
        self.P.add("dve", lambda e: e.tensor_tensor_scan(out=out, data0=d0, data1=d1, initial=init, op0=op0, op1=op1),
                   _rl(R), _rl(W))

    def recip(self, out, in_, R, W):
        self.P.add("dve", lambda e: e.reciprocal(out=out, in_=in_), _rl(R), _rl(W))

    def bn_stats(self, out, in_, R, W):
        self.P.add("dve", lambda e: e.bn_stats(out=out, in_=in_), _rl(R), _rl(W))

    def bn_aggr(self, out, in_, R, W):
        self.P.add("dve", lambda e: e.bn_aggr(out=out, in_=in_), _rl(R), _rl(W))


class Arena:
    def __init__(self, ap, cols):
        self.ap = ap
        self.cols = cols
        self.off = 0

    def reset(self):
        self.off = 0

    def alloc(self, cols, dtype=BF16, parts=128):
        n = cols * (1 if dtype == BF16 else 2)
        n = (n + 1) // 2 * 2
        assert self.off + n <= self.cols, (self.off, n, self.cols)
        v = self.ap[0:parts, self.off:self.off + n]
        self.off += n
        if dtype != BF16:
            v = v.bitcast(dtype)
        return Buf(v)

    def ring(self, k, cols, dtype=BF16, parts=128):
        return Ring([self.alloc(cols, dtype, parts) for _ in range(k)])


import os
SKIP = set(os.environ.get('KSKIP', '').split(','))
NFK = 13
NTS = 96
MOE_SORTED = os.environ.get('MOE', 'sorted') == 'sorted'
NFQ = 19
DBG = {}


def build(stage=99, nslot=NSLOT, nexp=32):
    nc = bass.Bass("TRN2", target_bir_lowering=False)

    def din(name, shape, dt=F32):
        return Buf(nc.dram_tensor(name, shape, dt, kind="ExternalInput").ap(), tracked=False)

    def dscr(name, shape, dt=BF16, dbg=False):
        kind = "ExternalOutput" if dbg else "Internal"
        return Buf(nc.dram_tensor(name, shape, dt, kind=kind).ap(), tracked=False)

    d_xT = din("xT", [1024, T])
    d_xo = din("xo", [TO, 1024])
    d_ca = din("ca", [128, T])
    d_sa = din("sa", [128, T])
    d_ci = din("ci", [128, T])
    d_si = din("si", [128, T])
    d_wfk = din("wfk", [1024, NFK * 128])
    d_wfq = din("wfq", [1024, NFQ * 128])
    d_wtk = din("wtk", [1024, 1280])
    d_wtq = din("wtq", [1024, 520])
    d_wg = din("wg", [17, 256])
    d_gn = din("gn", [1, 128])
    d_wout = din("wout", [1024, 1024])
    d_ln1g = din("ln1g", [1, 1024])
    d_ln1b = din("ln1b", [1, 1024])
    d_wr = din("wr", [1024, 36])
    d_brr = din("brr", [1, 36])
    d_wein = din("wein", [32, 1024, 1024])
    d_weout = din("weout", [32, 512, 1024])
    d_ln2g = din("ln2g", [1, 1024])
    d_ln2b = din("ln2b", [1, 1024])
    d_dummyb = din("dummyb", [128, 128])
    d_cmask = din("cmask", [128, 6 * 128])
    d_out = Buf(nc.dram_tensor("out", [TO, 1024], F32, kind="ExternalOutput").ap(), tracked=False)
    d_rc = din("rconst", [128, 2 * 128 + 2048 + 96 + 1 + 32])

    dbg = stage < 99
    s_KT = dscr("s_KT", [128, NSLOT, 4, 128], BF16, dbg and stage == 1)
    s_IKT = dscr("s_IKT", [96, T], BF16, dbg and stage == 1)
    s_V = dscr("s_V", [T, 520], BF16, dbg and stage == 1)
    s_QT = dscr("s_QT", [128, 32, 4, 128], BF16, dbg and stage == 1)
    s_IQT = dscr("s_IQT", [96, 32, 3, 128], BF16, dbg and stage == 1)
    s_IW = dscr("s_IW", [TO, 8], F32, dbg and stage == 1)
    s_KGT = dscr("s_KGT", [128, NSLOT, 2, 128], BF16, dbg and stage == 1)
    s_QGT = dscr("s_QGT", [128, 32, 2, 128], BF16, dbg and stage == 1)
    s_KD = dscr("s_KD", [T, 256], BF16, dbg and stage == 1)
    s_VG = dscr("s_VG", [T, 512], BF16, dbg and stage == 1)
    s_DEC = dscr("s_DEC", [128, 2, 128], F32, dbg and stage == 1)
    s_GS = dscr("s_GS", [TO, 512], F32, dbg and stage == 1)
    s_YT = dscr("s_YT", [128, 32, 8, 128], BF16, dbg and stage in (2, 3))
    s_THR = dscr("s_THR", [TO, 2], F32, dbg and stage == 3)
    s_H1 = dscr("s_H1", [TO, 1024], F32, dbg and stage == 4)
    s_H1T = dscr("s_H1T", [128, 8, TO], BF16, dbg and stage == 4)
    s_GATE = dscr("s_GATE", [TO, 32], F32, dbg and stage == 4)
    I32 = mybir.dt.int32
    s_H1B = dscr("s_H1B", [TO, 1024], BF16)
    s_WBI = dscr("s_WBI", [32 * 128, 8192], BF16)
    s_WBO = dscr("s_WBO", [32 * 128, 4096], BF16)
    s_TAB = Buf(nc.dram_tensor("s_TAB", [NTS * 128, 16], I32, kind="Internal").ap())
    s_WIDX = dscr("s_WIDX", [128, NTS], I32)
    s_Y2 = dscr("s_Y2", [2 * TO, 1024], F32)

    es = ExitStack()
    with es:
        P = Prog(nc)
        k = K(P)
        arena_t = es.enter_context(nc.sbuf_tensor("arena", [128, 104000], BF16))
        A = Arena(arena_t, 104000)
        banks_t = [es.enter_context(nc.psum_tensor(f"bank{i}", [128, 512], F32)) for i in range(8)]
        tab_t = [es.enter_context(nc.sbuf_tensor(f"tabt{i}", [128, 16], mybir.dt.int32)) for i in range(3)]

        def banks():
            return [Buf(b[:, :]) for b in banks_t]

        MUL, ADD, SUB = ALU.mult, ALU.add, ALU.subtract

        PB = banks()
        cm = A.alloc(6 * 128, F32)
        k.dma(cm[:, :], d_cmask[:, :], [d_cmask], [cm])
        ident_f = cm[:, 0:128]
        triinc = cm[:, 128:256]
        trirev = cm[:, 256:384]
        wfk = A.alloc(8 * NFK * 128)
        wfq = A.alloc(8 * NFQ * 128)
        wtk = A.alloc(8 * 1280)
        wtq = A.alloc(8 * 520)
        wfk_v = wfk.ap.rearrange("p (c f) -> p c f", c=8)
        wfq_v = wfq.ap.rearrange("p (c f) -> p c f", c=8)
        wtk_v = wtk.ap.rearrange("p (c f) -> p c f", c=8)
        wtq_v = wtq.ap.rearrange("p (c f) -> p c f", c=8)
        wg = A.alloc(256, BF16, 32)
        wgs = A.alloc(256, F32, 32)
        stg = A.ring(2, 1024, F32)
        cvt_i = [0]

        def load_w(dram, view, ncols, dst):
            for c in range(8):
                for c0 in range(0, ncols, 1024):
                    w_ = min(1024, ncols - c0)
                    s = stg.nxt()
                    k.dma(s[:, 0:w_], dram[c * 128:(c + 1) * 128, c0:c0 + w_], [dram], [s])
                    eng = "act" if cvt_i[0] % 2 == 0 else "dve"
                    cvt_i[0] += 1
                    k.cp(eng, view[:, c, c0:c0 + w_], s[:, 0:w_], [s], [dst])

        load_w(d_wfk, wfk_v, NFK * 128, wfk)
        load_w(d_wfq, wfq_v, NFQ * 128, wfq)
        load_w(d_wtk, wtk_v, 1280, wtk)
        load_w(d_wtq, wtq_v, 520, wtq)
        k.dma(wgs[0:17, :], d_wg[:, :], [d_wg], [wgs])
        k.cp("dve", wg[0:17, :], wgs[0:17, :], [wgs], [wg])
        gbc = A.alloc(512, F32)
        for h in range(4):
            k.dma(gbc[:, h * 128:(h + 1) * 128], d_gn.ap.partition_broadcast(128), [d_gn], [gbc])

        xs = A.alloc(8 * 512, F32)
        xb = A.ring(2, 8 * 512)
        tabs = [A.alloc(512, F32) for _ in range(4)]
        t1r = A.ring(2, 512, F32)
        t2r = A.ring(2, 512, F32)
        obr = A.ring(2, 512)
        kt4r = A.ring(2, 2048)
        qt4r = A.ring(2, 1024)
        iq3r = A.ring(2, 768)
        bkT = A.alloc(2 * 512, F32)
        bgT = A.alloc(512)
        k.memset("pool", bgT[0:32, :], 1.0, [bgT])
        decs = A.alloc(2 * 128, F32)
        decs_v = decs.ap.rearrange("p (f n) -> p f n", f=2)
        k.memset("pool", decs[:, :], 1.0, [decs])
        tz = A.alloc(256, F32)
        la = A.alloc(256, F32)
        erev = A.alloc(256, F32)
        e1 = A.alloc(256, F32)
        e2 = A.alloc(256, F32)
        vout = A.ring(2, 520)
        for b_ in vout.bufs:
            k.memset("pool", b_[:, :], 1.0, [b_])
        vgout = A.ring(2, 512)
        kdout = A.ring(2, 256)
        kgout = A.ring(2, 256)
        qgout = A.ring(2, 256)
        gsout = A.ring(2, 512, F32)
        sil = A.alloc(512, F32)
        iwout = A.ring(2, 8, F32)

        xTv = d_xT.ap.rearrange("(c p) t -> p c t", p=128)
        nblk = nslot // 4
        pi = [0]

        def feat_tile(wview, wbuf, ti, xbuf, xcols, ncol, bank, col0, M=128):
            xv = xbuf.ap.rearrange("p (c t) -> p c t", c=8)
            for c in range(8):
                k.mm(bank[0:M, col0:col0 + ncol], wview[:, c, ti * 128:ti * 128 + M], xv[:, c, xcols[0]:xcols[1]],
                     c == 0, c == 7, [wbuf, xbuf], [bank])

        def feat_tile_own(wview, wbuf, ti, xbuf, bank, M=128):
            xv4 = xbuf.ap.rearrange("p (c s t) -> p c s t", c=8, s=4)
            for c in range(8):
                k.mm(bank.ap[0:M, 0:256].rearrange("p (s t) -> p s t", s=2), wview[:, c, ti * 128:ti * 128 + M],
                     xv4[:, c, 1::2, :], c == 0, c == 7, [wbuf, xbuf], [bank])

        def rope_out(bP, bR, ncol, tc_, ts_, rows, scale, ob_ap, obuf):
            t1 = t1r.nxt()
            t2 = t2r.nxt()
            nseg = ncol // 128
            c_ap, s_ap = tc_[0:rows, 0:ncol], ts_[0:rows, 0:ncol]
            k.stt(t1[0:rows, 0:ncol], bP[0:rows, 0:ncol], scale, c_ap, MUL, MUL, [bP, tc_], [t1])
            k.stt(t2[0:rows, 0:ncol], bR[0:rows, 0:ncol], scale, s_ap, MUL, MUL, [bR, ts_], [t2])
            v = lambda b_: b_.ap[0:rows, 0:ncol].rearrange("p (s t) -> p s t", s=nseg)
            k.tt("pool", ob_ap, v(t1), v(t2), ADD, [t1, t2], [obuf])

        tabs2 = []
        for sb_ in stg.bufs:
            for hf in range(2):
                tb2 = Buf(sb_.ap[:, hf * 512:(hf + 1) * 512])
                tb2.res = sb_.res
                tabs2.append(tb2)
        tab_sets = [tabs, tabs2]

        def block_loads(Bn):
            cn = Bn * 512
            for c in range(8):
                k.dma(xs[:, c * 512:(c + 1) * 512], d_xT[c * 128:(c + 1) * 128, cn:cn + 512], [d_xT], [xs])
            for tb, dt_ in zip(tab_sets[Bn % 2], (d_ca, d_sa, d_ci, d_si)):
                k.dma(tb[:, :], dt_[:, cn:cn + 512], [dt_], [tb])

        block_loads(0)
        for B in range(nblk):
            c0 = B * 512
            tabs = tab_sets[B % 2]
            xbb = xb.nxt()
            k.cp("act", xbb[:, :], xs[:, :], [xs], [xbb])
            if B + 1 < nblk:
                block_loads(B + 1)
            kt4 = kt4r.nxt()
            kt4_v = kt4.ap.rearrange("p (s f t) -> p s f t", s=4, f=4)
            for i in range(4):
                bP, bR = PB[(pi[0] * 2) % 4], PB[(pi[0] * 2 + 1) % 4]
                pi[0] += 1
                feat_tile(wfk_v, wfk, i, xbb, (0, 512), 512, bP, 0)
                feat_tile(wfk_v, wfk, 4 + i, xbb, (0, 512), 512, bR, 0)
                rope_out(bP, bR, 512, tabs[0], tabs[1], 128, 1.0, kt4_v[:, :, i, :], kt4)
            k.dma(s_KT[:, 4 * B:4 * B + 4].rearrange("p s f t -> p (s f t)"), kt4[:, :], [kt4], [])
            bP, bR = PB[(pi[0] * 2) % 4], PB[(pi[0] * 2 + 1) % 4]
            pi[0] += 1
            feat_tile(wfk_v, wfk, 8, xbb, (0, 512), 512, bP, 0, M=96)
            feat_tile(wfk_v, wfk, 9, xbb, (0, 512), 512, bR, 0, M=96)
            ob = obr.nxt()
            rope_out(bP, bR, 512, tabs[2], tabs[3], 96, 1.0, ob.ap[0:96, 0:512].rearrange("p (s t) -> p s t", s=4), ob)
            k.dma(s_IKT[:, c0:c0 + 512], ob[0:96, 0:512], [ob], [])
            for ft in range(2):
                bP = PB[(pi[0] * 2) % 4]
                pi[0] += 1
                feat_tile(wfk_v, wfk, 10 + ft, xbb, (0, 512), 512, bP, 0)
                k.cp("act", bkT[:, ft * 512:(ft + 1) * 512], bP[:, :], [bP], [bkT])
            bP = PB[(pi[0] * 2) % 4]
            pi[0] += 1
            feat_tile(wfk_v, wfk, 12, xbb, (0, 512), 512, bP, 0, M=16)
            k.cp("act", bgT[0:16, :], bP[0:16, :], [bP], [bgT])
            own_cols = [(128, 256), (384, 512)] if 'q' not in SKIP else []

            def tv_own(tb, rows):
                return tb.ap.rearrange("p (s t) -> p s t", s=4)[0:rows, 1::2, :]

            oc0 = B * 256
            qt4 = qt4r.nxt()
            qt4_v = qt4.ap.rearrange("p (u f t) -> p u f t", u=2, f=4)
            iq3 = iq3r.nxt()
            iq3_v = iq3.ap.rearrange("p (u g t) -> p u g t", u=2, g=3)
            for i in range(4 if 'q' not in SKIP else 0):
                bP, bR = PB[(pi[0] * 2) % 4], PB[(pi[0] * 2 + 1) % 4]
                pi[0] += 1
                feat_tile_own(wfq_v, wfq, i, xbb, bP)
                feat_tile_own(wfq_v, wfq, 4 + i, xbb, bR)
                t1 = t1r.nxt()
                t2 = t2r.nxt()
                v3 = lambda b_, rows: b_.ap[0:rows, 0:256].rearrange("p (s t) -> p s t", s=2)
                k.stt(v3(t1, 128), v3(bP, 128), 0.125, tv_own(tabs[0], 128), MUL, MUL, [bP, tabs[0]], [t1])
                k.stt(v3(t2, 128), v3(bR, 128), 0.125, tv_own(tabs[1], 128), MUL, MUL, [bR, tabs[1]], [t2])
                k.tt("pool", qt4_v[:, :, i, :], v3(t1, 128), v3(t2, 128), ADD, [t1, t2], [qt4])
            if 'q' not in SKIP:
                k.dma(s_QT[:, 2 * B:2 * B + 2].rearrange("p u f t -> p (u f t)"), qt4[:, :], [qt4], [])
            for i in range(3 if 'q' not in SKIP else 0):
                bP, bR = PB[(pi[0] * 2) % 4], PB[(pi[0] * 2 + 1) % 4]
                pi[0] += 1
                feat_tile_own(wfq_v, wfq, 8 + i, xbb, bP, M=96)
                feat_tile_own(wfq_v, wfq, 11 + i, xbb, bR, M=96)
                t1 = t1r.nxt()
                t2 = t2r.nxt()
                k.stt(v3(t1, 96), v3(bP, 96), 1.0, tv_own(tabs[2], 96), MUL, MUL, [bP, tabs[2]], [t1])
                k.stt(v3(t2, 96), v3(bR, 96), 1.0, tv_own(tabs[3], 96), MUL, MUL, [bR, tabs[3]], [t2])
                k.tt("pool", v3(t1, 96), v3(t1, 96), v3(t2, 96), ADD, [t1, t2], [t1])
                bA = PB[(pi[0] * 2) % 4]
                pi[0] += 1
                feat_tile_own(wfq_v, wfq, 16 + i, xbb, bA, M=96)
                t3 = t2r.nxt()
                k.act(t3[0:96, 0:256], bA[0:96, 0:256], AF.Abs, [bA], [t3], scale=1.0 / 16.0)
                k.tt("pool", iq3_v[0:96, :, i, :], v3(t1, 96), v3(t3, 96), MUL, [t1, t3], [iq3])
            if 'q' not in SKIP:
                k.dma(s_IQT[:, 2 * B:2 * B + 2].rearrange("p u g t -> p (u g t)"), iq3[0:96, :], [iq3], [])
            xv = xbb.ap.rearrange("p (c t) -> p c t", c=8)
            for sl in range(4 if 'slot' not in SKIP else 0):
                s = B * 4 + sl
                cs = sl * 128
                r0 = s * 128
                bG, bH = PB[4], PB[5]
                k.mm(bG[:, 0:256], bgT[0:17, cs:cs + 128], wg[0:17, :], True, True, [bgT, wg], [bG])
                k.act(tz[:, :], bG[:, 0:256], AF.Exp, [bG], [tz], scale=-1.0)
                k.act(la[:, :], tz[:, :], AF.Ln, [tz], [la], bias=1.0)
                k.mm(bG[:, 256:512], trirev, la[:, :], True, True, [cm, la], [bG])
                k.act(erev[:, :], bG[:, 256:512], AF.Exp, [bG], [erev])
                for ft in range(2):
                    k.mm(bH[:, ft * 128:(ft + 1) * 128], la[:, ft * 128:(ft + 1) * 128], triinc, True, True, [la, cm], [bH])
                k.act(e1[:, :], bH[:, 0:256], AF.Exp, [bH], [e1])
                k.act(e2[:, :], bH[:, 0:256], AF.Exp, [bH], [e2], scale=-1.0)
                e1v = e1.ap.rearrange("p (f t) -> p f t", f=2)
                k.cp("pool", decs_v[:, :, 2 * s:2 * s + 2], e1v[:, :, 63::64], [e1], [decs])
                kg = kgout.nxt()
                bkv = bkT.ap.rearrange("p (f t) -> p f t", f=2)[:, :, cs:cs + 128]
                k.tt("dve", kg.ap.rearrange("p (f t) -> p f t", f=2), bkv, e2.ap.rearrange("p (f t) -> p f t", f=2),
                     MUL, [bkT, e2], [kg])
                k.dma(s_KGT[:, s].rearrange("p f t -> p (f t)"), kg[:, :], [kg], [])
                bV, bW, bK = PB[6], PB[7], PB[5]
                for c in range(8):
                    k.mm(bV[:, :], xv[:, c, cs:cs + 128], wtk_v[:, c, 0:512], c == 0, c == 7, [xbb, wtk], [bV])
                vo = vout.nxt()
                k.cp("act", vo.ap.rearrange("p (h d) -> p h d", h=8)[:, :, 0:64],
                     bV.ap.rearrange("p (h d) -> p h d", h=8), [bV], [vo])
                k.dma(s_V[r0:r0 + 128, :], vo[:, :], [vo], [s_V])
                for c in range(8):
                    k.mm(bW[:, :], xv[:, c, cs:cs + 128], wtk_v[:, c, 512:1024], c == 0, c == 7, [xbb, wtk], [bW])
                vg = vgout.nxt()
                k.cp("dve", vg[:, :], bW[:, :], [bW], [vg])
                k.dma(s_VG[r0:r0 + 128, :], vg[:, :], [vg], [s_VG])
                for c in range(8):
                    k.mm(bK[:, 256:512], xv[:, c, cs:cs + 128], wtk_v[:, c, 1024:1280], c == 0, c == 7, [xbb, wtk], [bK])
                kd = kdout.nxt()
                k.tt("dve", kd[:, :], bK[:, 256:512], erev[:, :], MUL, [bK, erev], [kd])
                k.dma(s_KD[r0:r0 + 128, :], kd[:, :], [kd], [s_KD])
                if sl % 2 == 1:
                    j = s // 2
                    o0 = j * 128
                    bQ = PB[6]
                    for ft in range(2):
                        feat_tile(wfq_v, wfq, 14 + ft, xbb, (cs, cs + 128), 128, bQ, ft * 128)
                    qg = qgout.nxt()
                    k.stt(qg[:, :], bQ[:, 0:256], 0.125, e1[:, :], MUL, MUL, [bQ, e1], [qg])
                    k.dma(s_QGT[:, j].rearrange("p f t -> p (f t)"), qg[:, :], [qg], [])
                    bBR = PB[7]
                    for c in range(8):
                        k.mm(bBR[:, :], xv[:, c, cs:cs + 128], wtq_v[:, c, 0:512], c == 0, c == 7, [xbb, wtq], [bBR])
                    k.act(sil[:, :], bBR[:, :], AF.Silu, [bBR], [sil])
                    gs = gsout.nxt()
                    k.tt("pool", gs[:, :], sil[:, :], gbc[:, :], MUL, [sil, gbc], [gs])
                    k.dma(s_GS[o0:o0 + 128, :], gs[:, :], [gs], [s_GS])
                    bIW = PB[6]
                    for c in range(8):
                        k.mm(bIW[:, 256:264], xv[:, c, cs:cs + 128], wtq_v[:, c, 512:520], c == 0, c == 7, [xbb, wtq], [bIW])
                    iw_ = iwout.nxt()
                    k.ts("dve", iw_[:, :], bIW[:, 256:264], 1.0 / 16.0, None, MUL, None, [bIW], [iw_])
                    k.dma(s_IW[o0:o0 + 128, :], iw_[:, :], [iw_], [s_IW])
        k.dma(s_DEC.ap.rearrange("p f n -> p (f n)"), decs[:, :], [decs], [s_DEC])
        P.barrier()
        if stage >= 2:
            build_phase2(nc, P, k, A, banks, nslot, locals())
        wflag = {}
        if stage >= 3:
            g3 = dict(locals())
            build_phase3(nc, P, k, A, banks, nslot, g3)
            wflag["wconv_done"] = g3.get("wconv_done", False)
        if stage >= 4:
            build_phase4(nc, P, k, A, banks, nslot, locals())
        if stage >= 5:
            if MOE_SORTED:
                g5 = dict(locals())
                g5["wconv_done"] = wflag.get("wconv_done", False)
                build_phase5s(nc, P, k, A, banks, nslot, g5)
            else:
                build_phase5(nc, P, k, A, banks, nslot, nexp, locals())
        if stage < 5:
            A.reset()
            z = A.alloc(1024, F32)
            k.memset("pool", z[:, :], 0.0, [z])
            k.dma(d_out[0:128, :], z[:, :], [z], [d_out], final=True)
        for op in P.dmas:
            op.signal = True
            P.outs.append(op)
        P.emit(es)
    return nc


def build_phase2(nc, P, k, A, banks, nslot, g):
    MUL, ADD = ALU.mult, ALU.add
    A.reset()
    PB = banks()
    d_cmask = g["d_cmask"]
    s_KD, s_VG, s_KGT, s_QGT, s_GS, s_DEC, s_YT = (g[n] for n in ("s_KD", "s_VG", "s_KGT", "s_QGT", "s_GS", "s_DEC", "s_YT"))
    cm = A.alloc(768, F32)
    k.dma(cm[:, :], d_cmask[:, :], [], [cm])
    atmask4 = A.alloc(512, F32)
    for h in range(4):
        k.cp("pool", atmask4[:, h * 128:(h + 1) * 128], cm[:, 384:512], [cm], [atmask4])
    identb = A.alloc(128)
    k.cp("dve", identb[:, :], cm[:, 0:128], [cm], [identb])
    dec = A.alloc(256, F32)
    dec_v = dec.ap.rearrange("p (f n) -> p f n", f=2)
    k.dma(dec[:, :], s_DEC.ap.rearrange("p f n -> p (f n)"), [], [dec])
    S = A.alloc(256, F32)
    Sa = A.alloc(256)
    Sb = A.alloc(256)
    qg0 = A.alloc(256)
    qg1 = A.alloc(256)
    for b_ in (S, Sa, Sb, qg0, qg1):
        k.memset("pool", b_[:, :], 0.0, [b_])
    v2 = lambda b_: b_.ap.rearrange("p (f t) -> p f t", f=2)
    S_v, Sa_v, Sb_v, qg0_v, qg1_v = v2(S), v2(Sa), v2(Sb), v2(qg0), v2(qg1)
    kgr = A.ring(2, 256)
    kdr = A.ring(2, 256)
    vgr = A.ring(2, 512)
    qgr = A.ring(2, 256)
    gsr = A.ring(2, 512, F32)
    atm = A.alloc(512)
    yb = A.alloc(512)
    ybT = A.ring(2, 512)
    junk = A.alloc(128, F32)
    ss = A.alloc(4, F32)
    ms = A.alloc(4, F32)
    lnv = A.alloc(4, F32)
    rstd = A.alloc(4, F32)
    bKV, bKV2, bAT, bO, bT, bAT2 = PB[0], PB[1], PB[2], PB[3], PB[4], PB[5]
    bT_bf = bT.ap[:, 0:256].bitcast(BF16)
    ld2 = {}

    def loads2(sn):
        rn = sn * 128
        kd_ = kdr.nxt()
        k.dma(kd_[:, :], s_KD[rn:rn + 128, :], [], [kd_])
        vg_ = vgr.nxt()
        k.dma(vg_[:, :], s_VG[rn:rn + 128, :], [], [vg_])
        kg_ = qg_ = gs_ = None
        if sn % 2 == 1:
            jn = sn // 2
            kg_ = kgr.nxt()
            k.dma(kg_[:, :], s_KGT[:, sn].rearrange("p f t -> p (f t)"), [], [kg_])
            qg_ = qgr.nxt()
            k.dma(qg_[:, :], s_QGT[:, jn].rearrange("p f t -> p (f t)"), [], [qg_])
            gs_ = gsr.nxt()
            k.dma(gs_[:, :], s_GS[jn * 128:(jn + 1) * 128, :], [], [gs_])
        ld2[sn] = (kd_, vg_, kg_, qg_, gs_)

    loads2(0)
    for s in range(nslot):
        own = s % 2 == 1
        j = s // 2
        r0 = s * 128
        o0 = j * 128
        if s + 1 < nslot:
            loads2(s + 1)
        kd_, vg_, kg_, qg_, gs_ = ld2.pop(s)
        for ft in range(2):
            k.mm(bKV[:, ft * 256:(ft + 1) * 256], kd_[0:64, ft * 128:(ft + 1) * 128], vg_[0:64, ft * 256:(ft + 1) * 256],
                 True, True, [kd_, vg_], [bKV])
        if own:
            for h in range(4):
                ft, pb = h // 2, 64 * (h % 2)
                bnk = bAT if h % 2 == 0 else bAT2
                k.mm(bnk[:, ft * 128:(ft + 1) * 128], v2(kg_)[pb:pb + 64, ft, :], v2(qg_)[pb:pb + 64, ft, :], True, True,
                     [kg_, qg_], [bnk])
            atm_v = atm.ap.rearrange("p (h t) -> p h t", h=4)
            am_v = atmask4.ap.rearrange("p (h t) -> p h t", h=4)[:, 0:2, :]
            for par, bnk in enumerate((bAT, bAT2)):
                k.tt("dve", atm_v[:, par::2, :], bnk.ap[:, 0:256].rearrange("p (h t) -> p h t", h=2), am_v, MUL,
                     [bnk, atmask4], [atm])
            k.cp("pool", qg0_v[:, :, 0:64], v2(qg_)[:, :, 0:64], [qg_], [qg0])
            k.cp("pool", qg1_v[:, :, 64:128], v2(qg_)[:, :, 64:128], [qg_], [qg1])
        for h in range(4):
            ft, pb = h // 2, 64 * (h % 2)
            k.stt(S_v[pb:pb + 64, ft, :], S_v[pb:pb + 64, ft, :], dec_v[pb:pb + 64, ft, 2 * s:2 * s + 1],
                  bKV[pb:pb + 64, ft * 256 + (h % 2) * 128: ft * 256 + (h % 2) * 128 + 128], MUL, ADD, [S, dec, bKV], [S])
        if own:
            k.cp("act", Sb[:, :], S[:, :], [S], [Sb])
        for ft in range(2):
            k.mm(bKV2[:, ft * 256:(ft + 1) * 256], kd_[64:128, ft * 128:(ft + 1) * 128], vg_[64:128, ft * 256:(ft + 1) * 256],
                 True, True, [kd_, vg_], [bKV2])
        if own:
            for h in range(4):
                ft, pb = h // 2, 64 * (h % 2)
                oc = bO[:, h * 128:(h + 1) * 128]
                k.mm(oc, atm[:, h * 128:(h + 1) * 128], vg_[:, h * 128:(h + 1) * 128], True, False, [atm, vg_], [bO])
                k.mm(oc, qg0_v[pb:pb + 64, ft, :], Sa_v[pb:pb + 64, ft, :], False, False, [qg0, Sa], [bO])
                k.mm(oc, qg1_v[pb:pb + 64, ft, :], Sb_v[pb:pb + 64, ft, :], False, True, [qg1, Sb], [bO])
            for h in range(4):
                k.act(junk[:, :], bO[:, h * 128:(h + 1) * 128], AF.Square, [bO], [junk, ss], accum=ss[:, h:h + 1])
            k.ts("dve", ms[:, :], ss[:, :], 1.0 / 128.0, EPS, MUL, ADD, [ss], [ms])
            k.act(lnv[:, :], ms[:, :], AF.Ln, [ms], [lnv])
            k.act(rstd[:, :], lnv[:, :], AF.Exp, [lnv], [rstd], scale=-0.5)
            for h in range(4):
                k.stt(yb[:, h * 128:(h + 1) * 128], bO[:, h * 128:(h + 1) * 128], rstd[:, h:h + 1],
                      gs_[:, h * 128:(h + 1) * 128], MUL, MUL, [bO, rstd, gs_], [yb])
            for h in range(4):
                k.tr(bT_bf[:, h * 128:(h + 1) * 128], yb[:, h * 128:(h + 1) * 128], identb[:, :], [yb, identb], [bT])
            yt = ybT.nxt()
            k.cp("act", yt[:, :], bT_bf[:, 0:512], [bT], [yt])
            k.dma(s_YT[:, j, 4:8, :].rearrange("p f t -> p (f t)"), yt[:, :], [yt], [])
        for h in range(4):
            ft, pb = h // 2, 64 * (h % 2)
            k.stt(S_v[pb:pb + 64, ft, :], S_v[pb:pb + 64, ft, :], dec_v[pb:pb + 64, ft, 2 * s + 1:2 * s + 2],
                  bKV2[pb:pb + 64, ft * 256 + (h % 2) * 128: ft * 256 + (h % 2) * 128 + 128], MUL, ADD, [S, dec, bKV2], [S])
        if not own:
            k.cp("act", Sa[:, :], S[:, :], [S], [Sa])
    P.barrier()


def build_phase3(nc, P, k, A, banks, nslot, g):
    MUL, ADD = ALU.mult, ALU.add
    A.reset()
    PB = banks()
    d_cmask, d_dummyb = g["d_cmask"], g["d_dummyb"]
    s_IKT, s_IQT, s_IW, s_QT, s_KT, s_V, s_YT = (g[n] for n in ("s_IKT", "s_IQT", "s_IW", "s_QT", "s_KT", "s_V", "s_YT"))
    cm = A.alloc(768, F32)
    k.dma(cm[:, :], d_cmask[:, :], [], [cm])
    identf = cm[:, 0:128]
    identb = A.alloc(128)
    k.cp("dve", identb[:, :], cm[:, 0:128], [cm], [identb])
    blockmask = cm[:, 512:640]
    sel64 = cm[:, 640:768]
    dmy = A.alloc(128, F32)
    k.dma(dmy[:, :], d_dummyb[:, :], [], [dmy])
    zl = A.alloc(128)
    k.memset("pool", zl[:, :], 0.0, [zl])
    zr = A.alloc(512)
    k.memset("pool", zr[:, :], 0.0, [zr])
    nkmax = nslot * 128
    ikt = A.alloc(nkmax)
    for c0 in range(0, nkmax, 2048):
        c1 = min(nkmax, c0 + 2048)
        k.dma(ikt[0:96, c0:c1], s_IKT[:, c0:c1], [], [ikt])
    scores = [A.alloc(nkmax, F32), A.alloc(nkmax, F32)]
    junk = A.alloc(nkmax)
    negsel = A.alloc(nkmax)
    nmT = A.alloc(nkmax)
    relu = A.ring(6, 512)
    iqr = A.ring(2, 384)
    iwr = A.ring(2, 8, F32)
    qr = A.ring(3, 1024)
    for b_ in qr.bufs:
        k.memset("pool", b_[:, :], 0.0, [b_])
    yTr = A.ring(2, 512)
    rd = A.alloc(8, F32)
    ktr = A.ring(4, 512)
    vtr = A.ring(4, 520)
    pTr = A.ring(3, 512)
    dgsr = A.ring(2, 8 * 128)
    sgr = A.ring(2, 8, F32)
    mid = A.alloc(2, F32)
    cnt = A.alloc(2, F32)
    sgn = A.alloc(2, F32)
    tq = A.alloc(2, F32)
    tq2 = A.alloc(2, F32)
    thr = A.alloc(2, F32)
    yar = A.ring(2, 512)
    cpi = [0]
    NITER = 15
    W0 = 32.0
    ACT_FRAC = float(os.environ.get("ACTFRAC", "0.0"))
    nj = nslot // 2
    st = {}

    def load(j):
        iq_ = iqr.nxt()
        iq_v = iq_.ap.rearrange("p (g t) -> p g t", g=3)
        k.dma(iq_[0:96, :], s_IQT[:, j].rearrange("p g t -> p (g t)"), [], [iq_])
        iw_ = iwr.nxt()
        k.dma(iw_[:, :], s_IW[j * 128:(j + 1) * 128, :], [], [iw_])
        q_ = qr.nxt()
        q_bd = q_.ap.rearrange("p (f u t) -> p f u t", f=4, u=2)
        k.dma(q_bd[0:64, :, 0, :], s_QT[0:64, j], [], [q_])
        k.dma(q_bd[64:128, :, 1, :], s_QT[64:128, j], [], [q_])
        sg = sgr.nxt()
        k.act(sg[:, :], iw_[:, :], AF.Sign, [iw_], [sg])
        dgs = dgsr.nxt()
        for h in range(8):
            k.ts("pool", dgs[:, h * 128:(h + 1) * 128], identf, sg[:, h:h + 1], None, MUL, None, [cm, sg], [dgs])
        st[j] = (iq_, dgs, q_)

    def indexer(j):
        iq_, dgs, q_ = st[j]
        score = scores[j % 2]
        iq_v = iq_.ap.rearrange("p (g t) -> p g t", g=3)
        nkeys = (2 * j + 2) * 128
        for kb in range(0, nkeys, 512):
            w = min(512, nkeys - kb)
            rs = []
            for h in range(8):
                gi, pb = h // 3, 32 * (h % 3)
                bank = PB[h % 3]
                k.mm(bank[:, 0:w], iq_v[pb:pb + 32, gi, :], ikt[pb:pb + 32, kb:kb + w], True, True, [iq_, ikt], [bank])
                r = relu.nxt()
                k.act(r[:, 0:w], bank[:, 0:w], AF.Relu, [bank], [r])
                rs.append(r)
                if h >= 1:
                    hp = h - 1
                    k.mm(PB[3][:, 0:w], dgs[:, hp * 128:(hp + 1) * 128], rs[hp][:, 0:w], hp == 0, False, [dgs, rs[hp]], [PB[3]])
                if h % 2 == 1:
                    yield
            k.mm(PB[3][:, 0:w], dgs[:, 7 * 128:8 * 128], rs[7][:, 0:w], False, True, [dgs, rs[7]], [PB[3]])
            eng = "act" if (kb // 512) % 2 == 0 else "dve"
            k.cp(eng, score[:, kb:kb + w], PB[3][:, 0:w], [PB[3]], [score])
            yield
        k.tt("dve", score[:, 0:128], score[:, 0:128], dmy[:, :], ADD, [score, dmy], [score])
        k.tt("dve", score[:, nkeys - 128:nkeys], score[:, nkeys - 128:nkeys], blockmask, ADD, [score, cm], [score])

    def idx_steps(j):
        nkeys = (2 * j + 2) * 128
        return ((nkeys + 511) // 512) * 5

    def select(j, gens, exhaust=None):
        score = scores[j % 2]
        nk = 2 * j + 2
        nkeys = nk * 128
        na = int(nkeys * ACT_FRAC) // 128 * 128
        nd = nkeys - na
        k.memset("dve", mid[:, :], 0.0, [mid])
        w_ = W0
        for it in range(NITER):
            k.ts("dve", junk[:, 0:nd], score[:, 0:nd], mid[:, 0:1], None, ALU.is_ge, ADD, [score, mid], [junk, cnt],
                 accum=cnt[:, 0:1])
            if na > 0:
                k.act(negsel[:, nd:nkeys], score[:, nd:nkeys], AF.Sign, [score, mid], [negsel, sgn], scale=-1.0,
                      bias=mid[:, 0:1], accum=sgn[:, 0:1])
                k.stt(tq[:, 0:1], sgn[:, 0:1], -0.5, cnt[:, 0:1], MUL, ADD, [sgn, cnt], [tq])
                k.ts("dve", tq2[:, 0:1], tq[:, 0:1], 255.5 - na / 2.0, w_ / 2, ALU.is_ge, MUL, [tq], [tq2])
            else:
                k.ts("dve", tq2[:, 0:1], cnt[:, 0:1], 255.5, w_ / 2, ALU.is_ge, MUL, [cnt], [tq2])
            k.stt(mid[:, 0:1], tq2[:, 0:1], -w_ / 4, mid[:, 0:1], ADD, ADD, [tq2, mid], [mid])
            w_ /= 2
            for (gen, steps) in gens:
                for _ in range((steps * (it + 1)) // NITER - (steps * it) // NITER):
                    next(gen, None)
        for (gen, steps) in (gens if exhaust is None else gens[:exhaust]):
            for _ in gen:
                pass
        k.ts("dve", thr[:, 0:1], mid[:, 0:1], -w_ / 2, None, ADD, None, [mid], [thr])
        k.ts("dve", negsel[:, 0:nkeys], score[:, 0:nkeys], thr[:, 0:1], NEG, ALU.is_lt, MUL, [score, thr], [negsel])
        for g0 in range(0, nk, 4):
            n = min(4, nk - g0)
            bank = PB[3]
            bank_bf = bank.ap[:, 0:256].bitcast(BF16)
            for u in range(n):
                kt = g0 + u
                k.tr(bank_bf[:, u * 128:(u + 1) * 128], negsel[:, kt * 128:(kt + 1) * 128], identb[:, :], [negsel, identb], [bank])
            eng = "act" if cpi[0] % 2 == 0 else "dve"
            cpi[0] += 1
            k.cp(eng, nmT[:, g0 * 128:(g0 + n) * 128], bank_bf[:, 0:n * 128], [bank], [nmT])

    def attend(j):
        iq_, dgs, q_ = st[j]
        q_bd = q_.ap.rearrange("p (f c) -> p f c", f=4)
        nk = 2 * j + 2
        bO = [PB[4], PB[5]]
        for hg in range(2):
            k.mm(bO[hg][:, 0:260], zl[:, 0:128], zr[:, 0:260], True, False, [zl, zr], [bO[hg]])
        tiles = {}

        def fetch(kt):
            kt_ = ktr.nxt()
            k.dma(kt_[:, :], s_KT[:, kt].rearrange("p f t -> p (f t)"), [], [kt_])
            vt_ = vtr.nxt()
            k.dma(vt_[:, :], s_V[kt * 128:(kt + 1) * 128, :], [], [vt_])
            tiles[kt] = (kt_, vt_)

        def qk(kt, hg):
            kt_, vt_ = tiles[kt]
            kt_v = kt_.ap.rearrange("p (f t) -> p f t", f=4)
            nm4 = nmT.ap[:, kt * 128:(kt + 1) * 128].unsqueeze(1).to_broadcast([128, 4, 128])
            bS = PB[6 + hg]
            k.mm(bS.ap.rearrange("p (h t) -> p h t", h=4), identb[:, :], nm4, True, False, [identb, nmT], [bS])
            for u in range(2):
                ft = hg * 2 + u
                k.mm(bS[:, u * 256:(u + 1) * 256], kt_v[:, ft, :], q_bd[:, ft, :], False, u == 1, [kt_, q_], [bS])
            p_ = pTr.nxt()
            k.act(p_[:, :], bS[:, :], AF.Exp, [bS], [p_])
            return p_

        def pv(kt, hg, p_):
            kt_, vt_ = tiles[kt]
            for hh in range(4):
                h = hg * 4 + hh
                k.mm(bO[hg][:, hh * 65:(hh + 1) * 65], p_[:, hh * 128:(hh + 1) * 128], vt_[:, h * 65:(h + 1) * 65],
                     False, (kt == nk - 1 and hh == 3), [p_, vt_], [bO[hg]])

        units = [(kt, hg) for kt in range(nk) for hg in range(2)]
        fetch(0)
        pend = None
        for ui, (kt, hg) in enumerate(units):
            if hg == 0 and kt + 1 < nk:
                fetch(kt + 1)
            p_ = qk(kt, hg)
            if pend is not None:
                pv(*pend)
            pend = (kt, hg, p_)
            if hg == 1:
                yield
        pv(*pend)
        ya_ = yar.nxt()
        for hg in range(2):
            ov = bO[hg].ap[:, 0:260].rearrange("p (h d) -> p h d", h=4)
            k.recip(rd[:, hg * 4:(hg + 1) * 4], ov[:, :, 64], [bO[hg]], [rd])
            for hh in range(4):
                h = hg * 4 + hh
                k.ts("dve", ya_[:, h * 64:(h + 1) * 64], ov[:, hh, 0:64], rd[:, h:h + 1], None, MUL, None, [bO[hg], rd], [ya_])
        bank = PB[3]
        bank_bf = bank.ap[:, 0:256].bitcast(BF16)
        for f in range(4):
            k.tr(bank_bf[:, f * 128:(f + 1) * 128], ya_[:, f * 128:(f + 1) * 128], identb[:, :], [ya_, identb], [bank])
        yT = yTr.nxt()
        k.cp("act", yT[:, :], bank_bf[:, 0:512], [bank], [yT])
        k.dma(s_YT[:, j, 0:4, :].rearrange("p f t -> p (f t)"), yT[:, :], [yT], [])

    wgen = None
    if MOE_SORTED and os.environ.get("WCONV", "p3") == "p3":
        print("phase3 arena used before wconv:", A.off)
        wgen = wconv_gen(k, A, g, engs=("pool",))
        g["wconv_done"] = True
    wsteps = 384
    load(0)
    for _ in indexer(0):
        pass
    for j in range(nj):
        gens = []
        if j + 1 < nj:
            load(j + 1)
            gens.append((indexer(j + 1), idx_steps(j + 1)))
        if j >= 1:
            gens.append((attend(j - 1), 2 * (j - 1) + 2))
        if wgen is not None:
            tot = nj * (nj + 1)
            gens.append((wgen, (wsteps * (j + 1) * (j + 2)) // tot - (wsteps * j * (j + 1)) // tot))
        select(j, gens, exhaust=len(gens) - (1 if wgen is not None else 0))
    for _ in attend(nj - 1):
        pass
    if wgen is not None:
        for _ in wgen:
            pass
    P.barrier()


def _bcast_load(k, A, d, n):
    b = A.alloc(n, F32)
    k.dma(b[:, :], d.ap.partition_broadcast(128), [], [b])
    return b


def _layernorm(k, A, tmp, sres, gbc, bbc, outb):
    MUL, ADD, SUB = ALU.mult, ALU.add, ALU.subtract
    st, mv, ve, lnv, rstd, hn = tmp
    for hf in range(2):
        k.bn_stats(st[:, hf * 6:(hf + 1) * 6], sres[:, hf * 512:(hf + 1) * 512], [sres], [st])
    k.bn_aggr(mv[:, 0:2], st[:, 0:12], [st], [mv])
    k.ts("dve", ve[:, 0:1], mv[:, 1:2], EPS, None, ADD, None, [mv], [ve])
    k.act(lnv[:, 0:1], ve[:, 0:1], AF.Ln, [ve], [lnv])
    k.act(rstd[:, 0:1], lnv[:, 0:1], AF.Exp, [lnv], [rstd], scale=-0.5)
    k.stt(ve[:, 1:2], mv[:, 0:1], -1.0, rstd[:, 0:1], MUL, MUL, [mv, rstd], [ve])
    k.act(hn[:, :], sres[:, :], AF.Identity, [sres, rstd, ve], [hn], scale=rstd[:, 0:1], bias=ve[:, 1:2])
    k.tt("dve", hn[:, :], hn[:, :], gbc[:, :], MUL, [hn, gbc], [hn])
    k.tt("dve", outb[:, :], hn[:, :], bbc[:, :], ADD, [hn, bbc], [outb])


def build_phase4(nc, P, k, A, banks, nslot, g):
    MUL, ADD, SUB = ALU.mult, ALU.add, ALU.subtract
    A.reset()
    PB = banks()
    d_cmask, d_wout, d_wr, d_brr, d_ln1g, d_ln1b, d_xo = (g[n] for n in ("d_cmask", "d_wout", "d_wr", "d_brr", "d_ln1g", "d_ln1b", "d_xo"))
    s_YT, s_H1, s_H1T, s_GATE = (g[n] for n in ("s_YT", "s_H1", "s_H1T", "s_GATE"))
    cm = A.alloc(768, F32)
    k.dma(cm[:, :], d_cmask[:, :], [], [cm])
    identf = cm[:, 0:128]
    wout = A.alloc(8 * 1024)
    wout_v = wout.ap.rearrange("p (c f) -> p c f", c=8)
    stg = A.ring(2, 2048, F32)
    for c in range(8):
        s_ = stg.nxt()
        k.dma(s_[:, 0:1024], d_wout[c * 128:(c + 1) * 128, :], [], [s_])
        k.cp("act" if c % 2 == 0 else "dve", wout_v[:, c, :], s_[:, 0:1024], [s_], [wout])
    wr = A.alloc(8 * 36, F32)
    wr_v = wr.ap.rearrange("p (c f) -> p c f", c=8)
    for c in range(8):
        k.dma(wr_v[:, c, :], d_wr[c * 128:(c + 1) * 128, :], [], [wr])
    brr = _bcast_load(k, A, d_brr, 36)
    g1 = _bcast_load(k, A, d_ln1g, 1024)
    b1 = _bcast_load(k, A, d_ln1b, 1024)
    ytr = A.ring(2, 1024)
    xor_ = A.ring(2, 1024, F32)
    sres_l = [A.alloc(1024, F32) for _ in range(2)]
    tmp_l = [(A.alloc(12, F32), A.alloc(2, F32), A.alloc(2, F32), A.alloc(2, F32), A.alloc(2, F32), A.alloc(1024, F32))
             for _ in range(2)]
    h1r = A.ring(2, 1024, F32)
    h1Tf_l = [A.alloc(1024, F32) for _ in range(2)]
    h1Tb = A.ring(2, 1024)
    lg_l = [A.alloc(36, F32) for _ in range(2)]
    sm_l = [[A.alloc(4, F32) for _ in range(12)] for _ in range(2)]
    em_l = [A.alloc(32, F32) for _ in range(2)]
    em2_l = [A.alloc(32, F32) for _ in range(2)]
    oh1_l = [A.alloc(32, F32) for _ in range(2)]
    oh2_l = [A.alloc(32, F32) for _ in range(2)]
    gater = A.ring(2, 32, F32)
    I32 = mybir.dt.int32
    if MOE_SORTED:
        d_rc = g["d_rc"]
        s_H1B, s_TAB, s_WIDX = g["s_H1B"], g["s_TAB"], g["s_WIDX"]
        rc = A.alloc(256 + 2048 + 96 + 1 + 32, F32)
        k.dma(rc[:, :], d_rc[:, :], [], [rc])
        LTb = A.alloc(128)
        onesb = A.alloc(128)
        k.cp("dve", LTb[:, :], rc[:, 0:128], [rc], [LTb])
        k.cp("dve", onesb[:, :], rc[:, 128:256], [rc], [onesb])
        thr_ap, iot, pcol, o32 = rc[:, 256:2304], rc[:, 2304:2400], rc[:, 2400:2401], rc[:, 2401:2433]
        Wall = A.alloc(1024, F32)
        Call = A.alloc(1024, F32)
        oh1all = A.alloc(1024, F32)
        oh2all = A.alloc(1024, F32)
        g1all = A.alloc(32, F32)
        g2all = A.alloc(32, F32)
        Crun = A.alloc(32, F32)
        for b_ in (Wall, Call, oh1all, oh2all, g1all, g2all, Crun):
            k.memset("pool", b_[:, :], 0.0, [b_])
        ohs = A.alloc(32)
        h1b = A.ring(2, 1024)
    for j in range(nslot // 2):
        o0 = j * 128
        sres, tmp, h1Tf, lg, sm = sres_l[j % 2], tmp_l[j % 2], h1Tf_l[j % 2], lg_l[j % 2], sm_l[j % 2]
        em, em2, oh1, oh2 = em_l[j % 2], em2_l[j % 2], oh1_l[j % 2], oh2_l[j % 2]
        if j == 0:
            ld4 = {}

            def loads4(jn):
                yt_ = ytr.nxt()
                k.dma(yt_[:, :], s_YT[:, jn].rearrange("p f t -> p (f t)"), [], [yt_])
                xo_ = xor_.nxt()
                k.dma(xo_[:, :], d_xo[jn * 128:(jn + 1) * 128, :], [], [xo_])
                ld4[jn] = (yt_, xo_)

            loads4(0)
        if j + 1 < nslot // 2:
            loads4(j + 1)
        yt, xo = ld4.pop(j)
        yt_v = yt.ap.rearrange("p (f t) -> p f t", f=8)
        for hf in range(2):
            for f in range(8):
                k.mm(PB[hf][:, :], yt_v[:, f, :], wout_v[:, f, hf * 512:(hf + 1) * 512], f == 0, f == 7, [yt, wout], [PB[hf]])
        for hf in range(2):
            k.stt(sres[:, hf * 512:(hf + 1) * 512], xo[:, hf * 512:(hf + 1) * 512], ALPHA, PB[hf][:, :], MUL, ADD,
                  [xo, PB[hf]], [sres])
        h1 = h1r.nxt()
        _layernorm(k, A, tmp, sres, g1, b1, h1)
        k.dma(s_H1[o0:o0 + 128, :], h1[:, :], [h1], [])
        for c in range(8):
            k.tr(PB[2 + c // 4][:, (c % 4) * 128:(c % 4 + 1) * 128], h1[:, c * 128:(c + 1) * 128], identf, [h1, cm], [PB[2 + c // 4]])
        k.cp("act", h1Tf[:, 0:512], PB[2][:, :], [PB[2]], [h1Tf])
        k.cp("dve", h1Tf[:, 512:1024], PB[3][:, :], [PB[3]], [h1Tf])
        if not MOE_SORTED:
            hb = h1Tb.nxt()
            k.cp("pool", hb[:, :], h1Tf[:, :], [h1Tf], [hb])
            for c in range(8):
                k.dma(s_H1T[:, c, o0:o0 + 128], hb[:, c * 128:(c + 1) * 128], [hb], [])
        else:
            hb16 = h1b.nxt()
            k.cp("pool", hb16[:, :], h1[:, :], [h1], [hb16])
            k.dma(s_H1B[o0:o0 + 128, :], hb16[:, :], [hb16], [])
        h1Tf_v = h1Tf.ap.rearrange("p (c t) -> p c t", c=8)
        for c in range(8):
            k.mm(PB[4][:, 0:36], h1Tf_v[:, c, :], wr_v[:, c, :], c == 0, c == 7, [h1Tf, wr], [PB[4]])
        k.tt("dve", lg[:, :], PB[4][:, 0:36], brr[:, :], ADD, [PB[4], brr], [lg])
        gm, ngm, gsum, pg, m1, m2, dd, ee, p1, g1v, g2v, pen = sm
        k.reduce(gm[:, 0:1], lg[:, 0:4], ALU.max, [lg], [gm])
        ohg = oh2
        k.ts("dve", oh2[:, 0:4], lg[:, 0:4], gm[:, 0:1], None, ALU.is_ge, None, [lg, gm], [oh2])
        k.ts("dve", ngm[:, 0:1], gm[:, 0:1], -1.0, None, MUL, None, [gm], [ngm])
        k.act(em2[:, 0:4], lg[:, 0:4], AF.Exp, [lg, ngm], [em2, gsum], bias=ngm[:, 0:1], accum=gsum[:, 0:1])
        k.recip(pg[:, 0:1], gsum[:, 0:1], [gsum], [pg])
        k.ts("dve", pen[:, 0:4], oh2[:, 0:4], 1.0, 1e9, SUB, MUL, [oh2], [pen])
        for gi in range(4):
            k.ts("dve", em[:, gi * 8:(gi + 1) * 8], lg[:, 4 + gi * 8:4 + (gi + 1) * 8], pen[:, gi:gi + 1], None, ADD, None,
                 [lg, pen], [em])
        k.reduce(m1[:, 0:1], em[:, :], ALU.max, [em], [m1])
        k.ts("dve", oh1[:, :], em[:, :], m1[:, 0:1], None, ALU.is_ge, None, [em, m1], [oh1])
        k.stt(em2[:, :], oh1[:, :], -1e9, em[:, :], MUL, ADD, [oh1, em], [em2])
        k.reduce(m2[:, 0:1], em2[:, :], ALU.max, [em2], [m2])
        k.ts("dve", oh2[:, :], em2[:, :], m2[:, 0:1], None, ALU.is_ge, None, [em2, m2], [oh2])
        k.tt("dve", dd[:, 0:1], m2[:, 0:1], m1[:, 0:1], SUB, [m1, m2], [dd])
        k.act(ee[:, 0:1], dd[:, 0:1], AF.Exp, [dd], [ee])
        k.ts("dve", ee[:, 0:1], ee[:, 0:1], 1.0, None, ADD, None, [ee], [ee])
        k.recip(p1[:, 0:1], ee[:, 0:1], [ee], [p1])
        k.tt("dve", g1v[:, 0:1], p1[:, 0:1], pg[:, 0:1], MUL, [p1, pg], [g1v])
        k.tt("dve", g2v[:, 0:1], pg[:, 0:1], g1v[:, 0:1], SUB, [pg, g1v], [g2v])
        gt = gater.nxt()
        k.ts("dve", gt[:, :], oh1[:, :], g1v[:, 0:1], None, MUL, None, [oh1, g1v], [gt])
        k.stt(gt[:, :], oh2[:, :], g2v[:, 0:1], gt[:, :], MUL, ADD, [oh2, g2v, gt], [gt])
        if not MOE_SORTED:
            k.dma(s_GATE[o0:o0 + 128, :], gt[:, :], [gt], [])
        else:
            k.tt("dve", ohs[:, :], oh1[:, :], oh2[:, :], ADD, [oh1, oh2], [ohs])
            k.mm(PB[5][:, 0:32], LTb[:, :], ohs[:, :], True, True, [LTb, ohs], [PB[5]])
            k.cp("dve", Wall[:, j * 32:(j + 1) * 32], PB[5][:, 0:32], [PB[5]], [Wall])
            k.cp("pool", Call[:, j * 32:(j + 1) * 32], Crun[:, :], [Crun], [Call])
            k.mm(PB[5][:, 32:64], onesb[:, :], ohs[:, :], True, True, [onesb, ohs], [PB[5]])
            k.tt("dve", Crun[:, :], Crun[:, :], PB[5][:, 32:64], ADD, [Crun, PB[5]], [Crun])
            k.cp("pool", oh1all[:, j * 32:(j + 1) * 32], oh1[:, :], [oh1], [oh1all])
            k.cp("pool", oh2all[:, j * 32:(j + 1) * 32], oh2[:, :], [oh2], [oh2all])
            k.cp("pool", g1all[:, j:j + 1], g1v[:, 0:1], [g1v], [g1all])
            k.cp("pool", g2all[:, j:j + 1], g2v[:, 0:1], [g2v], [g2all])
    if MOE_SORTED:
        ntile = nslot // 2
        v3 = lambda b_: b_.ap.rearrange("p (a b) -> p a b", a=32)
        cmpt = A.alloc(2048, F32)
        k.tt("dve", v3(cmpt), thr_ap.rearrange("p (a b) -> p a b", a=32), Crun.ap.unsqueeze(2).to_broadcast([128, 32, 64]),
             ALU.is_lt, [rc, Crun], [cmpt])
        ceil_ = A.alloc(32, F32)
        k.reduce(ceil_[:, :], v3(cmpt), ALU.add, [cmpt], [ceil_])
        padded = A.alloc(32, F32)
        k.ts("dve", padded[:, :], ceil_[:, :], 128.0, None, MUL, None, [ceil_], [padded])
        incl = A.alloc(32, F32)
        k.scan(incl[:, :], o32, padded[:, :], 0.0, MUL, ADD, [rc, padded], [incl])
        offs = A.alloc(32, F32)
        k.tt("dve", offs[:, :], incl[:, :], padded[:, :], SUB, [incl, padded], [offs])
        endt = A.alloc(32, F32)
        k.ts("dve", endt[:, :], incl[:, :], 1.0 / 128.0, None, MUL, None, [incl], [endt])
        texp = A.alloc(NTS, F32)
        k.memset("dve", texp[:, :], 0.0, [texp])
        for e in range(32):
            k.stt(texp[:, :], iot, endt[:, e:e + 1], texp[:, :], ALU.is_ge, ADD, [rc, endt, texp], [texp])
        k.ts("dve", texp[:, :], texp[:, :], 31.0, None, ALU.min, None, [texp], [texp])
        widxf = A.alloc(NTS, F32)
        k.ts("dve", widxf[:, :], texp[:, :], 128.0, pcol, MUL, ADD, [texp, rc], [widxf])
        same = A.alloc(NTS, F32)
        k.memset("dve", same[:, :], 0.0, [same])
        k.tt("dve", same[:, 2:NTS], texp[:, 2:NTS], texp[:, 0:NTS - 2], ALU.is_equal, [texp], [same])
        k.stt(widxf[:, :], same[:, :], 4096.0, widxf[:, :], MUL, ADD, [same, widxf], [widxf])
        widx = A.alloc(NTS, I32)
        k.cp("dve", widx[:, :], widxf[:, :], [widxf], [widx])
        k.dma(s_WIDX.ap, widx[:, :], [widx], [])
        roff = A.alloc(1024, F32)
        k.tt("dve", roff[:, :], Wall[:, :], Call[:, :], ADD, [Wall, Call], [roff])
        k.tt("dve", v3(roff), v3(roff), offs.ap.unsqueeze(1).to_broadcast([128, 32, 32]), ADD, [roff, offs], [roff])
        tmpm = A.alloc(1024, F32)
        posf = A.alloc(64, F32)
        k.tt("dve", tmpm[:, :], roff[:, :], oh1all[:, :], MUL, [roff, oh1all], [tmpm])
        k.reduce(posf[:, 0:32], v3(tmpm), ALU.add, [tmpm], [posf])
        k.tt("dve", tmpm[:, :], roff[:, :], oh2all[:, :], MUL, [roff, oh2all], [tmpm])
        k.reduce(posf[:, 32:64], v3(tmpm), ALU.add, [tmpm], [posf])
        posi = A.alloc(64, I32)
        k.cp("dve", posi[:, :], posf[:, :], [posf], [posi])
        ent = A.alloc(64 * 16, I32)
        k.memset("pool", ent[:, :], 0, [ent])
        tokf = A.alloc(32, F32)
        k.ts("dve", tokf[:, :], iot[:, 0:32], 128.0, pcol, MUL, ADD, [rc], [tokf])
        tokf2 = A.alloc(32, F32)
        k.ts("dve", tokf2[:, :], tokf[:, :], float(TO), None, ADD, None, [tokf], [tokf2])
        ent_v = ent.ap.rearrange("p (k c) -> p k c", c=16)
        entf_v = ent.ap.bitcast(F32).rearrange("p (k c) -> p k c", c=16)
        k.cp("dve", ent_v[:, 0:32, 0], tokf[:, :], [tokf], [ent])
        k.cp("dve", ent_v[:, 32:64, 0], tokf[:, :], [tokf], [ent])
        td = A.alloc(32, F32)
        for (c0_, src_, add_) in ((0, tokf, 0.0), (32, tokf2, 0.0)):
            k.ts("dve", td[:, :], src_[:, :], 2.0, None, MUL, None, [src_], [td])
            k.cp("dve", ent_v[:, c0_:c0_ + 32, 1], td[:, :], [td], [ent])
            k.ts("dve", td[:, :], td[:, :], 1.0, None, ADD, None, [td], [td])
            k.cp("dve", ent_v[:, c0_:c0_ + 32, 3], td[:, :], [td], [ent])
        k.cp("dve", entf_v[:, 0:32, 2], g1all[:, :], [g1all], [ent])
        k.cp("dve", entf_v[:, 32:64, 2], g2all[:, :], [g2all], [ent])
        tabi = A.alloc(NTS * 16, I32)
        k.memset("pool", tabi[:, :], 0, [tabi])
        k.memset("pool", tabi.ap.rearrange("p (t c) -> p t c", c=16)[:, :, 1], 1000000, [tabi])
        k.memset("pool", tabi.ap.rearrange("p (t c) -> p t c", c=16)[:, :, 3], 1000000, [tabi])
        k.dma(s_TAB.ap.rearrange("(p t) c -> p (t c)", p=128), tabi[:, :], [tabi], [s_TAB])
        for sl_ in range(2):
            for jj in range(ntile):
                kk = sl_ * 32 + jj
                k.scatter(s_TAB.ap[:, :], ent_v[:, kk, :], posi[:, kk:kk + 1], NTS * 128 - 1, [ent, posi, s_TAB], [])
    P.barrier()


def build_phase5(nc, P, k, A, banks, nslot, nexp, g):
    MUL, ADD = ALU.mult, ALU.add
    A.reset()
    PB = banks()
    d_wein, d_weout, d_ln2g, d_ln2b, d_out = (g[n] for n in ("d_wein", "d_weout", "d_ln2g", "d_ln2b", "d_out"))
    s_H1, s_H1T, s_GATE = (g[n] for n in ("s_H1", "s_H1T", "s_GATE"))
    g2 = _bcast_load(k, A, d_ln2g, 1024)
    b2 = _bcast_load(k, A, d_ln2b, 1024)
    ntile = nslot // 2
    TB = min(16, ntile)
    nblk = ntile // TB
    acc = A.alloc(TB * 1024, F32)
    acc_v = acc.ap.rearrange("p (t f) -> p t f", t=TB)
    hT = A.alloc(8 * TB * 128)
    hT_v = hT.ap.rearrange("p (c t) -> p c t", c=8)
    gt = A.alloc(TB * 32, F32)
    gt_v = gt.ap.rearrange("p (t e) -> p t e", t=TB)
    winr = A.ring(2, 8 * 1024)
    woutr = A.ring(2, 4 * 1024)
    stg = A.ring(3, 1024, F32)
    aTr = A.ring(2, 4 * 512)
    silr = A.ring(2, 512, F32)
    h1r = A.ring(1, 1024, F32)
    sres = A.alloc(1024, F32)
    tmp = (A.alloc(12, F32), A.alloc(2, F32), A.alloc(2, F32), A.alloc(2, F32), A.alloc(2, F32), A.alloc(1024, F32))
    outr = A.ring(1, 1024, F32)
    ybank = [0]
    for blk in range(nblk):
        t0 = blk * TB * 128
        for c in range(8):
            k.dma(hT_v[:, c, :], s_H1T[:, c, t0:t0 + TB * 128], [], [hT])
        for t in range(TB):
            k.dma(gt_v[:, t, :], s_GATE[t0 + t * 128:t0 + (t + 1) * 128, :], [], [gt])
        for e in range(nexp):
            win = winr.nxt()
            win_v = win.ap.rearrange("p (c f) -> p c f", c=8)
            for c in range(8):
                s_ = stg.nxt()
                k.dma(s_[:, :], d_wein.ap[e][c * 128:(c + 1) * 128, :], [], [s_])
                k.cp("pool", win_v[:, c, :], s_[:, :], [s_], [win])
            wo = woutr.nxt()
            wo_v = wo.ap.rearrange("p (c f) -> p c f", c=4)
            for c in range(4):
                s_ = stg.nxt()
                k.dma(s_[:, :], d_weout.ap[e][c * 128:(c + 1) * 128, :], [], [s_])
                k.cp("pool", wo_v[:, c, :], s_[:, :], [s_], [wo])
            for sb in range(0, TB, 4):
                nt = min(4, TB - sb)
                ncol = nt * 128
                aT = aTr.nxt()
                aT_v = aT.ap.rearrange("p (f t) -> p f t", f=4)
                for ft in range(4):
                    bg_, bu_ = PB[(ft % 2) * 2], PB[(ft % 2) * 2 + 1]
                    for c in range(8):
                        k.mm(bg_[:, 0:ncol], win_v[:, c, ft * 128:(ft + 1) * 128], hT_v[:, c, sb * 128:sb * 128 + ncol],
                             c == 0, c == 7, [win, hT], [bg_])
                    for c in range(8):
                        k.mm(bu_[:, 0:ncol], win_v[:, c, 512 + ft * 128:512 + (ft + 1) * 128], hT_v[:, c, sb * 128:sb * 128 + ncol],
                             c == 0, c == 7, [win, hT], [bu_])
                    sl_ = silr.nxt()
                    k.act(sl_[:, 0:ncol], bg_[:, 0:ncol], AF.Silu, [bg_], [sl_])
                    k.tt("dve", aT_v[:, ft, 0:ncol], sl_[:, 0:ncol], bu_[:, 0:ncol], MUL, [sl_, bu_], [aT])
                for u in range(nt):
                    t = sb + u
                    yb0, yb1 = PB[4 + (ybank[0] % 2) * 2], PB[5 + (ybank[0] % 2) * 2]
                    ybank[0] += 1
                    for hf, yb_ in enumerate((yb0, yb1)):
                        for fc in range(4):
                            k.mm(yb_[:, :], aT_v[:, fc, u * 128:(u + 1) * 128], wo_v[:, fc, hf * 512:(hf + 1) * 512],
                                 fc == 0, fc == 3, [aT, wo], [yb_])
                    for hf, yb_ in enumerate((yb0, yb1)):
                        dst = acc_v[:, t, hf * 512:(hf + 1) * 512]
                        if e == 0:
                            k.ts("dve", dst, yb_[:, :], gt_v[:, t, e:e + 1], None, MUL, None, [yb_, gt], [acc])
                        else:
                            k.stt(dst, yb_[:, :], gt_v[:, t, e:e + 1], dst, MUL, ADD, [yb_, gt, acc], [acc])
        for t in range(TB):
            r0 = t0 + t * 128
            h1 = h1r.nxt()
            k.dma(h1[:, :], s_H1[r0:r0 + 128, :], [], [h1])
            k.stt(sres[:, :], h1[:, :], ALPHA, acc_v[:, t, :], MUL, ADD, [h1, acc], [sres])
            ob = outr.nxt()
            _layernorm(k, A, tmp, sres, g2, b2, ob)
            k.dma(d_out[r0:r0 + 128, :], ob[:, :], [ob], [], final=True)


def wconv_gen(k, A, g, engs=("act", "dve", "pool")):
    d_wein, d_weout, s_WBI, s_WBO = g["d_wein"], g["d_weout"], g["s_WBI"], g["s_WBO"]
    stg = A.ring(3, 1024, F32)
    stb = A.ring(3, 1024)
    i = 0
    for e in range(32):
        for (dsrc, ddst, nch) in ((d_wein, s_WBI, 8), (d_weout, s_WBO, 4)):
            for c in range(nch):
                s_ = stg.nxt()
                b_ = stb.nxt()
                k.dma(s_[:, :], dsrc.ap[e][c * 128:(c + 1) * 128, :], [], [s_])
                k.cp(engs[i % len(engs)], b_[:, :], s_[:, :], [s_], [b_])
                i += 1
                k.dma(ddst[e * 128:(e + 1) * 128, c * 1024:(c + 1) * 1024], b_[:, :], [b_], [])
                yield


def build_phase5s(nc, P, k, A, banks, nslot, g):
    MUL, ADD = ALU.mult, ALU.add
    I32 = mybir.dt.int32
    A.reset()
    PB = banks()
    d_cmask, d_ln2g, d_ln2b, d_out = (g[n] for n in ("d_cmask", "d_ln2g", "d_ln2b", "d_out"))
    s_H1, s_H1B, s_WBI, s_WBO, s_TAB, s_WIDX, s_Y2 = (g[n] for n in ("s_H1", "s_H1B", "s_WBI", "s_WBO", "s_TAB", "s_WIDX", "s_Y2"))
    if not g.get("wconv_done"):
        for _ in wconv_gen(k, A, g):
            pass
        P.barrier()
        A.reset()
    cm = A.alloc(768, F32)
    k.dma(cm[:, :], d_cmask[:, :], [], [cm])
    identb = A.alloc(128)
    k.cp("dve", identb[:, :], cm[:, 0:128], [cm], [identb])
    g2 = _bcast_load(k, A, d_ln2g, 1024)
    b2 = _bcast_load(k, A, d_ln2b, 1024)
    widx = A.alloc(NTS, I32)
    k.dma(widx[:, :], s_WIDX.ap, [], [widx])
    tabr = Ring([Buf(t_[:, :]) for t_ in g["tab_t"]])
    xsr = A.ring(2, 1024)
    winr = A.ring(2, 8192)
    wor = A.ring(2, 4096)
    xTr = A.ring(2, 1024)
    silr = A.ring(2, 512, F32)
    ar = A.ring(2, 512)
    aTr = A.ring(2, 512)
    ysr = A.ring(2, 1024, F32)
    ntile = nslot // 2
    nts = min(NTS, 2 * ntile + 32)
    staged = {}

    def fetch(t):
        tab = tabr.nxt()
        k.dma(tab[:, :], s_TAB.ap[t * 128:(t + 1) * 128, :], [s_TAB], [tab])
        xs = xsr.nxt()
        k.gather(xs[:, :], s_H1B.ap[0:(nslot // 2) * 128, :], tab[:, 0:1], [tab], [xs])
        win = winr.nxt()
        k.gather(win[:, :], s_WBI.ap[:, :], widx[:, t:t + 1], [widx], [win], bound=32 * 128 - 1)
        wo = wor.nxt()
        k.gather(wo[:, :], s_WBO.ap[:, :], widx[:, t:t + 1], [widx], [wo], bound=32 * 128 - 1)
        staged[t] = (tab, xs, win, wo)

    fetch(0)
    for t in range(nts):
        if t + 1 < nts:
            fetch(t + 1)
        tab, xs, win, wo = staged.pop(t)
        win_v = win.ap.rearrange("p (c f) -> p c f", c=8)
        wo_v = wo.ap.rearrange("p (c f) -> p c f", c=4)
        bT = PB[0]
        bT_bf = bT.ap.bitcast(BF16)
        for c in range(8):
            k.tr(bT_bf[:, c * 128:(c + 1) * 128], xs[:, c * 128:(c + 1) * 128], identb[:, :], [xs, identb], [bT])
        xT = xTr.nxt()
        k.cp("act", xT[:, 0:512], bT_bf[:, 0:512], [bT], [xT])
        k.cp("dve", xT[:, 512:1024], bT_bf[:, 512:1024], [bT], [xT])
        xT_v = xT.ap.rearrange("p (c t) -> p c t", c=8)
        bg_, bu_ = PB[1 + 2 * (t % 2)], PB[2 + 2 * (t % 2)]
        for c in range(8):
            k.mm(bg_[:, :], xT_v[:, c, :], win_v[:, c, 0:512], c == 0, c == 7, [xT, win], [bg_])
        for c in range(8):
            k.mm(bu_[:, :], xT_v[:, c, :], win_v[:, c, 512:1024], c == 0, c == 7, [xT, win], [bu_])
        sl_ = silr.nxt()
        k.act(sl_[:, :], bg_[:, :], AF.Silu, [bg_], [sl_])
        a_ = ar.nxt()
        gate = tab.ap[:, 2:3].bitcast(F32)
        k.stt(a_[:, :], sl_[:, :], gate, bu_[:, :], MUL, MUL, [sl_, tab, bu_], [a_])
        bA = PB[5]
        bA_bf = bA.ap[:, 0:256].bitcast(BF16)
        for fc in range(4):
            k.tr(bA_bf[:, fc * 128:(fc + 1) * 128], a_[:, fc * 128:(fc + 1) * 128], identb[:, :], [a_, identb], [bA])
        aT = aTr.nxt()
        k.cp("act", aT[:, :], bA_bf[:, 0:512], [bA], [aT])
        aT_v = aT.ap.rearrange("p (f t) -> p f t", f=4)
        ys = ysr.nxt()
        for hf in range(2):
            by = PB[6 + hf]
            for fc in range(4):
                k.mm(by[:, :], aT_v[:, fc, :], wo_v[:, fc, hf * 512:(hf + 1) * 512], fc == 0, fc == 3, [aT, wo], [by])
            k.cp("act" if hf == 0 else "dve", ys[:, hf * 512:(hf + 1) * 512], by[:, :], [by], [ys])
        y2v = s_Y2.ap.rearrange("r (h f) -> (r h) f", h=2)
        k.scatter(y2v, ys[:, 0:512], tab[:, 1:2], 4 * TO - 1, [ys, tab], [])
        k.scatter(y2v, ys[:, 512:1024], tab[:, 3:4], 4 * TO - 1, [ys, tab], [])
    P.barrier()
    h1r = A.ring(2, 1024, F32)
    y2r = A.ring(2, 2048, F32)
    sres_l = [A.alloc(1024, F32) for _ in range(2)]
    tmp_l = [(A.alloc(12, F32), A.alloc(2, F32), A.alloc(2, F32), A.alloc(2, F32), A.alloc(2, F32), A.alloc(1024, F32))
             for _ in range(2)]
    outr = A.ring(2, 1024, F32)
    ld5 = {}

    def loads5(tn):
        rn = tn * 128
        h1_ = h1r.nxt()
        k.dma(h1_[:, :], s_H1[rn:rn + 128, :], [], [h1_])
        y2_ = y2r.nxt()
        k.dma(y2_[:, 0:1024], s_Y2[rn:rn + 128, :], [], [y2_])
        k.dma(y2_[:, 1024:2048], s_Y2[TO + rn:TO + rn + 128, :], [], [y2_])
        ld5[tn] = (h1_, y2_)

    loads5(0)
    for t in range(ntile):
        r0 = t * 128
        sres, tmp = sres_l[t % 2], tmp_l[t % 2]
        if t + 1 < ntile:
            loads5(t + 1)
        h1, y2 = ld5.pop(t)
        k.stt(sres[:, :], h1[:, :], ALPHA, y2[:, 0:1024], MUL, ADD, [h1, y2], [sres])
        k.tt("pool", sres[:, :], sres[:, :], y2[:, 1024:2048], ADD, [sres, y2], [sres])
        ob = outr.nxt()
        _layernorm(k, A, tmp, sres, g2, b2, ob)
        k.dma(d_out[r0:r0 + 128, :], ob[:, :], [ob], [], final=True)


_OFF = np.cumsum([0, 512, 512, 512, 256, 32, 8, 256, 256, 512, 512, 16])
O_AQ, O_AK, O_AV, O_IQ, O_IK, O_IW, O_BQ, O_BK, O_BV, O_BR, O_BG = [int(v) for v in _OFF[:11]]


def _consts():
    j = np.arange(128)[:, None]
    i = np.arange(128)[None, :]
    same = (j // 64) == (i // 64)
    ident = np.eye(128, dtype=np.float32)
    triinc = np.where(same & (j <= i), -1.0 / 16.0, 0.0).astype(np.float32)
    trirev = np.where(same & (j > i), -1.0 / 16.0, 0.0).astype(np.float32)
    atmask = np.where(same & (j <= i), 1.0, 0.0).astype(np.float32)
    blockmask = np.where((j < 64) & (i >= 64), -1e30, 0.0).astype(np.float32)
    sel64 = np.zeros((128, 128), np.float32)
    sel64[64, :64] = 1.0
    return np.concatenate([ident, triinc, trirev, atmask, blockmask, sel64], axis=1)


def _rconsts():
    j = np.arange(128)[:, None]
    i = np.arange(128)[None, :]
    lt = (j < i).astype(np.float32)
    ones = np.ones((128, 128), np.float32)
    thr = np.tile((np.arange(64, dtype=np.float32) * 128.0)[None, :], (32, 1)).reshape(1, 2048)
    thr = np.tile(thr, (128, 1))
    iot = np.tile(np.arange(96, dtype=np.float32)[None, :], (128, 1))
    pcol = np.arange(128, dtype=np.float32)[:, None]
    o32 = np.ones((128, 32), np.float32)
    return np.ascontiguousarray(np.concatenate([lt, ones, thr, iot, pcol, o32], axis=1))


def _weight_layouts(w_in):
    w = np.asarray(w_in[0], np.float32)
    z = np.zeros((1024, 128), np.float32)

    def tile(cols):
        t = z.copy()
        t[:, :len(cols)] = w[:, cols]
        return t

    def rot(base, nh, dh):
        cols = []
        for h in range(nh):
            b = base + h * dh
            cols += list(range(b + dh // 2, b + dh)) + list(range(b, b + dh // 2))
        return cols

    fk = []
    for i in range(4):
        fk.append(tile(list(range(O_AK + i * 128, O_AK + (i + 1) * 128))))
    for i in range(4):
        fk.append(tile(rot(O_AK + i * 128, 2, 64)))
    ik = list(range(O_IK, O_IK + 32))
    fk.append(tile(ik * 3))
    fk.append(tile(rot(O_IK, 1, 32) * 3))
    for ft in range(2):
        fk.append(tile(list(range(O_BK + ft * 128, O_BK + (ft + 1) * 128))))
    fk.append(tile(list(range(O_BG, O_BG + 16))))
    fq = []
    for i in range(4):
        fq.append(tile(list(range(O_AQ + i * 128, O_AQ + (i + 1) * 128))))
    for i in range(4):
        fq.append(tile(rot(O_AQ + i * 128, 2, 64)))
    groups = [(0, 3), (3, 3), (6, 2)]
    for (h0, n) in groups:
        fq.append(tile(list(range(O_IQ + h0 * 32, O_IQ + (h0 + n) * 32))))
    for (h0, n) in groups:
        fq.append(tile(rot(O_IQ + h0 * 32, n, 32)))
    for ft in range(2):
        fq.append(tile(list(range(O_BQ + ft * 128, O_BQ + (ft + 1) * 128))))
    for (h0, n) in groups:
        cols = []
        for h in range(h0, h0 + n):
            cols += [O_IW + h] * 32
        fq.append(tile(cols))
    wfk = np.ascontiguousarray(np.concatenate(fk, axis=1))
    wfq = np.ascontiguousarray(np.concatenate(fq, axis=1))
    wtk = np.ascontiguousarray(np.concatenate([w[:, O_AV:O_AV + 512], w[:, O_BV:O_BV + 512], w[:, O_BK:O_BK + 256]], axis=1))
    wtq = np.ascontiguousarray(np.concatenate([w[:, O_BR:O_BR + 512], w[:, O_IW:O_IW + 8]], axis=1))
    return wfk, wfq, wtk, wtq


def _rope_tables(pos):
    pos = pos.astype(np.float32)
    inv32 = (1.0 / (np.float32(10000.0) ** (np.arange(32, dtype=np.float32) / np.float32(32)))).astype(np.float32)
    inv16 = (1.0 / (np.float32(10000.0) ** (np.arange(16, dtype=np.float32) / np.float32(16)))).astype(np.float32)
    p = np.arange(128)
    angA = (pos[None, :] * inv32[(p % 64) % 32][:, None]).astype(np.float32)
    sgnA = np.where((p % 64) < 32, -1.0, 1.0).astype(np.float32)[:, None]
    angI = (pos[None, :] * inv16[(p % 32) % 16][:, None]).astype(np.float32)
    sgnI = np.where((p % 32) < 16, -1.0, 1.0).astype(np.float32)[:, None]
    return (np.cos(angA).astype(np.float32), (np.sin(angA) * sgnA).astype(np.float32),
            np.cos(angI).astype(np.float32), (np.sin(angI) * sgnI).astype(np.float32))


def prep_inputs(inputs, nslot=NSLOT):
    x = np.asarray(inputs["x"], np.float32)
    wfk, wfq, wtk, wtq = _weight_layouts(inputs["w_in"])
    wg = np.concatenate([np.asarray(inputs["w_gla_gate"][0], np.float32),
                         np.asarray(inputs["b_gla_gate"], np.float32).reshape(1, 256)], axis=0)
    wr = np.concatenate([np.asarray(inputs["w_group_router"][0], np.float32),
                         np.asarray(inputs["w_expert_router"][0], np.float32)], axis=1)
    brr = np.concatenate([np.asarray(inputs["b_group_router"], np.float32).reshape(1, 4),
                          np.asarray(inputs["b_expert_router"], np.float32).reshape(1, 32)], axis=1)
    common = {
        "wfk": wfk, "wfq": wfq, "wtk": wtk, "wtq": wtq, "wg": np.ascontiguousarray(wg),
        "gn": np.asarray(inputs["g_gla_norm"], np.float32).reshape(1, 128),
        "wout": np.ascontiguousarray(inputs["w_out"][0], dtype=np.float32),
        "ln1g": np.asarray(inputs["ln1_g"], np.float32).reshape(1, 1024),
        "ln1b": np.asarray(inputs["ln1_b"], np.float32).reshape(1, 1024),
        "wr": np.ascontiguousarray(wr), "brr": brr,
        "wein": np.ascontiguousarray(inputs["w_expert_in"][0], dtype=np.float32),
        "weout": np.ascontiguousarray(inputs["w_expert_out"][0], dtype=np.float32),
        "ln2g": np.asarray(inputs["ln2_g"], np.float32).reshape(1, 1024),
        "ln2b": np.asarray(inputs["ln2_b"], np.float32).reshape(1, 1024),
        "cmask": _consts(),
        "rconst": _rconsts(),
    }
    maps = []
    for c in range(8):
        b, half = c // 2, c % 2
        xb = x[b]
        if half == 0:
            xs = np.concatenate([np.zeros((128, 1024), np.float32), xb[:T - 128]], axis=0)
            pos = np.concatenate([np.zeros(128), np.arange(T - 128)])
        else:
            xs = xb
            pos = np.arange(T)
        ca, sa, ci, si = _rope_tables(pos)
        xo = xs.reshape(NSLOT, 128, 1024)[1::2].reshape(TO, 1024)
        m = dict(common)
        m["xT"] = np.ascontiguousarray(xs.T)
        m["xo"] = np.ascontiguousarray(xo)
        m["ca"], m["sa"], m["ci"], m["si"] = ca, sa, ci, si
        m["dummyb"] = np.full((128, 128), -1e30 if half == 0 else 0.0, np.float32)
        maps.append(m)
    return maps


def assemble(outs):
    y = np.zeros((4, T, 1024), np.float32)
    for c in range(8):
        b, half = c // 2, c % 2
        o = np.asarray(outs[c], np.float32).reshape(32, 128, 1024)
        yv = y[b].reshape(NSLOT, 128, 1024)
        if half == 0:
            yv[0::2] = o
        else:
            yv[1::2] = o
    return y


_NC_CACHE = {}


def kernel(**inputs):
    maps = prep_inputs(inputs)
    if "nc" not in _NC_CACHE:
        _NC_CACHE["nc"] = build()
    res = run_bass_kernel_spmd(_NC_CACHE["nc"], maps, core_ids=list(range(8)))
    return assemble([r["out"] for r in res.results])
```
